# Optimizing a Trainium2 kernel written in Bass

```python
import math
import jax, jax.numpy as jnp
from jax import lax
import numpy as np

D_MODEL = 1024
BATCH = 8
SEQ = 4096
DEPTH = 2

HEAD_DIM = 64
N_HEADS_FOX = 4
N_HEADS_DIL = 6
N_HEADS_SB = 6
W_FOX = N_HEADS_FOX * HEAD_DIM
W_DIL = N_HEADS_DIL * HEAD_DIM
W_SB = N_HEADS_SB * HEAD_DIM
MIX_WIDTH = W_FOX + W_DIL + W_SB
IN_COLS = 4 * W_FOX + N_HEADS_FOX + 3 * W_DIL + 3 * W_SB
Q_BLOCK = 128
DIL_PATTERNS = ((128, 1), (512, 4), (2048, 16))
FORGET_BIAS_INIT = 2.0
NUM_BUCKETS = 32
MAX_DISTANCE = 2048
N_EXPERTS = 16
N_GROUPS = 4
EXPERTS_PER_GROUP = N_EXPERTS // N_GROUPS
TOP_K = 2
D_FF_EXPERT = 512
RMS_EPS = 1e-6

kernel_name = "hybrid_fox_dilated_stickbreak_grouped_moe"


def rms_norm(x, gain):
    xf = x.astype(jnp.float32)
    y = xf * lax.rsqrt(jnp.mean(xf * xf, axis=-1, keepdims=True) + RMS_EPS)
    return (y * gain.astype(jnp.float32)).astype(x.dtype)


def split_heads(t, n_heads):
    return t.reshape(t.shape[:-1] + (n_heads, HEAD_DIM))


def t5_causal_bucket(distance):
    max_exact = NUM_BUCKETS // 2
    d = jnp.maximum(distance, max_exact).astype(jnp.float32)
    large = max_exact + (jnp.log(d / max_exact) / math.log(MAX_DISTANCE / max_exact)
                         * (NUM_BUCKETS - max_exact)).astype(jnp.int32)
    large = jnp.minimum(large, NUM_BUCKETS - 1)
    return jnp.where(distance < max_exact, distance, large)


def forgetting_attention(q, k, v, log_f):
    b_, s_, h_, e_ = q.shape
    nb = s_ // Q_BLOCK
    scale = e_ ** -0.5
    cum = jnp.cumsum(log_f.astype(jnp.float32), axis=1).transpose(0, 2, 1)
    q_blocks = q.reshape(b_, nb, Q_BLOCK, h_, e_).transpose(1, 0, 2, 3, 4)
    c_blocks = cum.reshape(b_, h_, nb, Q_BLOCK).transpose(2, 0, 1, 3)
    starts = jnp.arange(nb, dtype=jnp.int32) * Q_BLOCK
    key_pos = jnp.arange(s_, dtype=jnp.int32)

    def one_block(args):
        q_blk, c_blk, start = args
        logits = jnp.einsum('bqhe,bkhe->bhqk', q_blk, k).astype(jnp.float32) * scale
        logits = logits + c_blk[..., None] - cum[:, :, None, :]
        q_pos = start + jnp.arange(Q_BLOCK, dtype=jnp.int32)
        causal = key_pos[None, :] <= q_pos[:, None]
        p = jax.nn.softmax(jnp.where(causal, logits, -jnp.inf), axis=-1)
        return jnp.einsum('bhqk,bkhe->bqhe', p.astype(v.dtype), v)

    out = lax.map(one_block, (q_blocks, c_blocks, starts))
    return out.transpose(1, 0, 2, 3, 4).reshape(b_, s_, h_, e_)


def stick_breaking_attention(q, k, v):
    b_, s_, h_, e_ = q.shape
    nb = s_ // Q_BLOCK
    scale = e_ ** -0.5
    q_blocks = q.reshape(b_, nb, Q_BLOCK, h_, e_).transpose(1, 0, 2, 3, 4)
    starts = jnp.arange(nb, dtype=jnp.int32) * Q_BLOCK
    key_pos = jnp.arange(s_, dtype=jnp.int32)

    def one_block(args):
        q_blk, start = args
        z = jnp.einsum('bqhe,bkhe->bhqk', q_blk, k).astype(jnp.float32) * scale
        q_pos = start + jnp.arange(Q_BLOCK, dtype=jnp.int32)
        strict = key_pos[None, :] < q_pos[:, None]
        log_keep = jnp.where(strict, jax.nn.log_sigmoid(-z), 0.0)
        later = lax.cumsum(log_keep, axis=3, reverse=True) - log_keep
        a = jnp.where(strict, jnp.exp(jax.nn.log_sigmoid(z) + later), 0.0)
        return jnp.einsum('bhqk,bkhe->bqhe', a.astype(v.dtype), v)

    out = lax.map(one_block, (q_blocks, starts))
    return out.transpose(1, 0, 2, 3, 4).reshape(b_, s_, h_, e_)


def dilated_window_attention(q, k, v, rel_bias):
    b_, s_, h_, e_ = q.shape
    scale = e_ ** -0.5
    outs, lses = [], []
    q_loc = jnp.arange(Q_BLOCK, dtype=jnp.int32)
    k_loc = jnp.arange(2 * Q_BLOCK, dtype=jnp.int32)
    steps = q_loc[:, None] + Q_BLOCK - k_loc[None, :]
    for window, dil in DIL_PATTERNS:
        span = window // dil
        unit = dil * Q_BLOCK
        s_pad = -(-s_ // unit) * unit
        m = s_pad // dil
        nb = m // Q_BLOCK

        def to_blocks(t):
            t = jnp.pad(t, ((0, 0), (0, s_pad - s_), (0, 0), (0, 0)))
            t = t.reshape(b_, m, dil, h_, e_).transpose(0, 2, 3, 1, 4)
            return t.reshape(b_, dil, h_, nb, Q_BLOCK, e_)

        def with_prev(t):
            prev = jnp.pad(t, ((0, 0), (0, 0), (0, 0), (1, 0), (0, 0), (0, 0)))[:, :, :, :-1]
            return jnp.concatenate([prev, t], axis=4)

        qb = to_blocks(q)
        kw = with_prev(to_blocks(k))
        vw = with_prev(to_blocks(v))
        logits = jnp.einsum('brhnqe,brhnke->brhnqk', qb, kw).astype(jnp.float32) * scale
        bias = rel_bias[t5_causal_bucket(jnp.maximum(steps, 0) * dil)].transpose(2, 0, 1)
        band = (steps >= 0) & (steps <= span)
        key_exists = (jnp.arange(nb)[:, None] > 0) | (k_loc[None, :] >= Q_BLOCK)
        mask = band[None, :, :] & key_exists[:, None, :]
        logits = jnp.where(mask, logits + bias[:, None].astype(jnp.float32), -jnp.inf)
        lse = jax.nn.logsumexp(logits, axis=-1)
        p = jnp.exp(logits - lse[..., None])
        o = jnp.einsum('brhnqk,brhnke->brhnqe', p.astype(v.dtype), vw)
        o = o.reshape(b_, dil, h_, m, e_).transpose(0, 3, 1, 2, 4).reshape(b_, s_pad, h_, e_)[:, :s_]
        lse = lse.reshape(b_, dil, h_, m).transpose(0, 3, 1, 2).reshape(b_, s_pad, h_)[:, :s_]
        outs.append(o)
        lses.append(lse)
    weights = jax.nn.softmax(jnp.stack(lses, axis=0), axis=0)
    return jnp.sum(weights[..., None].astype(v.dtype) * jnp.stack(outs, axis=0), axis=0)


def hybrid_mixer(h, w_in, b_forget, q_gain_fox, k_gain_fox, q_gain_dil, k_gain_dil, rel_bias, w_out):
    b_, s_, _ = h.shape
    proj = jnp.einsum('bsd,dc->bsc', h, w_in)
    sizes = [W_FOX, W_FOX, W_FOX, W_FOX, N_HEADS_FOX, W_DIL, W_DIL, W_DIL, W_SB, W_SB, W_SB]
    points = [int(p) for p in np.cumsum(sizes)[:-1]]
    q_f, k_f, v_f, g_f, f_logit, q_d, k_d, v_d, q_s, k_s, v_s = jnp.split(proj, points, axis=-1)
    log_f = jax.nn.log_sigmoid(f_logit.astype(jnp.float32) + b_forget.astype(jnp.float32))
    o_fox = forgetting_attention(rms_norm(split_heads(q_f, N_HEADS_FOX), q_gain_fox),
                                 rms_norm(split_heads(k_f, N_HEADS_FOX), k_gain_fox),
                                 split_heads(v_f, N_HEADS_FOX), log_f)
    o_fox = o_fox.reshape(b_, s_, W_FOX) * jax.nn.sigmoid(g_f)
    o_dil = dilated_window_attention(rms_norm(split_heads(q_d, N_HEADS_DIL), q_gain_dil),
                                     rms_norm(split_heads(k_d, N_HEADS_DIL), k_gain_dil),
                                     split_heads(v_d, N_HEADS_DIL), rel_bias).reshape(b_, s_, W_DIL)
    o_sb = stick_breaking_attention(split_heads(q_s, N_HEADS_SB), split_heads(k_s, N_HEADS_SB),
                                    split_heads(v_s, N_HEADS_SB)).reshape(b_, s_, W_SB)
    mixed = jnp.concatenate([o_fox, o_dil, o_sb], axis=-1)
    return jnp.einsum('bsc,cd->bsd', mixed, w_out)


def grouped_moe(h, router_w, router_b, w_gate, w_up, w_down):
    b_, s_, d_ = h.shape
    tokens = h.reshape(-1, d_)
    probs = jax.nn.softmax((tokens @ router_w).astype(jnp.float32) + router_b.astype(jnp.float32), axis=-1)
    grouped = probs.reshape(-1, N_GROUPS, EXPERTS_PER_GROUP)
    group_score = lax.top_k(grouped, TOP_K)[0].sum(-1)
    best_group = jnp.argmax(group_score, axis=-1)
    in_group = jnp.take_along_axis(grouped, best_group[:, None, None], axis=1)[:, 0]
    top_p, top_local = lax.top_k(in_group, TOP_K)
    expert_idx = best_group[:, None] * EXPERTS_PER_GROUP + top_local
    gates = top_p / jnp.sum(top_p, axis=-1, keepdims=True)
    combine = jnp.sum(jax.nn.one_hot(expert_idx, N_EXPERTS, dtype=jnp.float32) * gates[..., None], axis=1)
    combine = combine.astype(tokens.dtype)
    out = jnp.zeros_like(tokens)
    for e in range(N_EXPERTS):
        hid = jax.nn.silu(tokens @ w_gate[e]) * (tokens @ w_up[e])
        out = out + combine[:, e:e + 1] * (hid @ w_down[e])
    return out.reshape(b_, s_, d_)


def setup_inputs(seed: int = 0) -> dict:
    key = jax.random.key(seed)
    ks = jax.random.split(key, 19)
    f32 = jnp.float32

    def nrm(k, shape, scale):
        return jax.random.normal(k, shape, f32) * scale

    return {
        'x': nrm(ks[0], (BATCH, SEQ, D_MODEL), 1.0),
        'c': nrm(ks[1], (BATCH, D_MODEL), 1.0),
        'ada_w': nrm(ks[2], (DEPTH, D_MODEL, 6 * D_MODEL), 0.5 * D_MODEL ** -0.5),
        'ada_b': nrm(ks[3], (DEPTH, 6 * D_MODEL), 0.02),
        'norm_mix': 1.0 + nrm(ks[4], (DEPTH, D_MODEL), 0.02),
        'norm_ffn': 1.0 + nrm(ks[5], (DEPTH, D_MODEL), 0.02),
        'w_in': nrm(ks[6], (DEPTH, D_MODEL, IN_COLS), D_MODEL ** -0.5),
        'b_forget': FORGET_BIAS_INIT + nrm(ks[7], (DEPTH, N_HEADS_FOX), 0.1),
        'q_gain_fox': 1.0 + nrm(ks[8], (DEPTH, HEAD_DIM), 0.02),
        'k_gain_fox': 1.0 + nrm(ks[9], (DEPTH, HEAD_DIM), 0.02),
        'q_gain_dil': 1.0 + nrm(ks[10], (DEPTH, HEAD_DIM), 0.02),
        'k_gain_dil': 1.0 + nrm(ks[11], (DEPTH, HEAD_DIM), 0.02),
        'rel_bias': nrm(ks[12], (NUM_BUCKETS, N_HEADS_DIL), 0.2),
        'w_out': nrm(ks[13], (DEPTH, MIX_WIDTH, D_MODEL), MIX_WIDTH ** -0.5),
        'router_w': nrm(ks[14], (D_MODEL, N_EXPERTS), D_MODEL ** -0.5),
        'router_b': nrm(ks[15], (N_EXPERTS,), 0.01),
        'w_gate': nrm(ks[16], (DEPTH, N_EXPERTS, D_MODEL, D_FF_EXPERT), D_MODEL ** -0.5),
        'w_up': nrm(ks[17], (DEPTH, N_EXPERTS, D_MODEL, D_FF_EXPERT), D_MODEL ** -0.5),
        'w_down': nrm(ks[18], (DEPTH, N_EXPERTS, D_FF_EXPERT, D_MODEL), D_FF_EXPERT ** -0.5),
    }


def reference(x, c, ada_w, ada_b, norm_mix, norm_ffn, w_in, b_forget, q_gain_fox, k_gain_fox,
              q_gain_dil, k_gain_dil, rel_bias, w_out, router_w, router_b, w_gate, w_up, w_down):
    cond = jax.nn.silu(c)
    for layer in range(DEPTH):
        mod = jnp.einsum('bd,de->be', cond, ada_w[layer]) + ada_b[layer]
        shift_m, scale_m, gate_m, shift_f, scale_f, gate_f = jnp.split(mod[:, None, :], 6, axis=-1)
        h = rms_norm(x, norm_mix[layer]) * (1.0 + scale_m) + shift_m
        x = x + gate_m * hybrid_mixer(h, w_in[layer], b_forget[layer], q_gain_fox[layer], k_gain_fox[layer],
                                      q_gain_dil[layer], k_gain_dil[layer], rel_bias, w_out[layer])
        h = rms_norm(x, norm_ffn[layer]) * (1.0 + scale_f) + shift_f
        x = x + gate_f * grouped_moe(h, router_w, router_b, w_gate[layer], w_up[layer], w_down[layer])
    return x
```

```python
import bisect
import contextlib
import numpy as np
import concourse.bass as bass
import concourse.mybir as mybir
from concourse.bass_utils import run_bass_kernel_spmd

F32 = mybir.dt.float32
BF16 = mybir.dt.bfloat16
AF = mybir.ActivationFunctionType
ALU = mybir.AluOpType
AX = mybir.AxisListType

D = 1024
DEPTH = 2
HD = 64
NCORES = 8
IN_COLS = 3332
NEXP = 16
DFF = 512
EPS = 1e-6
NEG = -30000.0


class Buf:
    __slots__ = ("name", "w", "r", "sem", "semv", "semk")

    def __init__(self, name):
        self.name = name
        self.w = None
        self.r = {}
        self.sem = None
        self.semv = 0
        self.semk = None


class Eng:
    def __init__(self, name, eng, sem):
        self.name = name
        self.eng = eng
        self.sem = sem
        self.seq = 0
        self.nsig = 0
        self.sig_seq = []
        self.sig_idx = []
        self.last = None
        self.seen = {}


class PEProxy:
    def __init__(self, eng):
        self.eng = eng
        self.stop = False

    def matmul(self, *a, **kw):
        self.stop = bool(kw.get("stop"))
        return self.eng.matmul(*a, **kw)

    def transpose(self, *a, **kw):
        self.stop = False
        return self.eng.transpose(*a, **kw)


class Sched:
    def __init__(self, nc, es):
        self.nc = nc
        self.es = es
        self.E = {}
        for name, eng in (("pe", nc.tensor), ("act", nc.scalar), ("dve", nc.vector),
                          ("pool", nc.gpsimd), ("sp", nc.sync)):
            sem = es.enter_context(nc.semaphore("sem_" + name))
            self.E[name] = Eng(name, eng, sem)
        self.pep = PEProxy(nc.tensor)
        self.bufs = {}
        self.nsem = 0
        self.sem_pool = {}

    def buf(self, name):
        return Buf(name)

    def _resolve(self, en, seq):
        E = self.E[en]
        i = bisect.bisect_left(E.sig_seq, seq)
        if i < len(E.sig_seq):
            return E.sem, E.sig_idx[i]
        assert E.seq >= seq and E.last is not None
        E.nsig += 1
        E.last.then_inc(E.sem, 1)
        E.sig_seq.append(E.seq)
        E.sig_idx.append(E.nsig)
        return E.sem, E.nsig

    def _wait(self, en, toks):
        C = self.E[en]
        emax = {}
        dmax = {}
        for t in toks:
            if t is None:
                continue
            if t[0] == "E":
                if t[1] == "pe" and en == "pe":
                    continue
                if emax.get(t[1], 0) < t[2]:
                    emax[t[1]] = t[2]
            else:
                k = t[1].name
                if k not in dmax or dmax[k][1] < t[2]:
                    dmax[k] = (t[1], t[2])
        waits = []
        for pn, seq in emax.items():
            sem, val = self._resolve(pn, seq)
            waits.append((sem.name, sem, val))
        for k, (b, val) in dmax.items():
            waits.append((b.sem.name, b.sem, val))
        for key, sem, val in waits:
            if C.seen.get(key, 0) < val:
                C.eng.wait_ge(sem, val)
                C.seen[key] = val

    @staticmethod
    def _collect(reads, writes):
        toks = []
        for b in reads:
            toks.append(b.w)
        for b in writes:
            toks.append(b.w)
            toks.extend(b.r.values())
        return toks

    def _mark(self, tok, key, reads, writes):
        for b in reads:
            b.r[key] = tok
            self.bufs[id(b)] = b
        for b in writes:
            b.w = tok
            b.r = {}
            self.bufs[id(b)] = b

    def op(self, en, fn, reads=(), writes=(), sig=None):
        self._wait(en, self._collect(reads, writes))
        E = self.E[en]
        if en == "pe":
            self.pep.stop = False
            ins = fn(self.pep)
            if sig is None:
                sig = self.pep.stop
        else:
            ins = fn(E.eng)
        E.seq += 1
        E.last = ins
        if sig or (sig is None and en != "pe"):
            E.nsig += 1
            ins.then_inc(E.sem, 1)
            E.sig_seq.append(E.seq)
            E.sig_idx.append(E.nsig)
        self._mark(("E", en, E.seq), "E" + en, reads, writes)
        return ins

    def dma(self, qn, out, in_, reads=(), writes=(), fn=None, nowaw=False, **kw):
        toks = self._collect(reads, writes)
        if nowaw:
            b0 = writes[0]
            toks = [t for t in toks if not (t is not None and t[0] == "D" and t[1] is b0 and t is b0.w)]
        self._wait(qn, toks)
        E = self.E[qn]
        b0 = writes[0]
        kind = "sw" if qn == "pool" else "hw"
        if b0.sem is not None and b0.semk != kind:
            raise AssertionError("buffer %s written by both SW and HW DMA queues" % b0.name)
        if b0.sem is None:
            b0.semk = kind
            pool = self.sem_pool.setdefault(kind, [])
            if pool:
                b0.sem, b0.semv = pool.pop()
            else:
                self.nsem += 1
                b0.sem = self.es.enter_context(self.nc.semaphore("dsem%d" % self.nsem))
                b0.semv = 0
        ins = fn(E.eng) if fn is not None else E.eng.dma_start(out=out, in_=in_, **kw)
        b0.semv += 16
        ins.then_inc(b0.sem, 16)
        self._mark(("D", b0, b0.semv), "D" + b0.sem.name, reads, writes)
        return ins

    def barrier(self):
        toks = []
        for b in self.bufs.values():
            toks.append(b.w)
            toks.extend(b.r.values())
        for en, E in self.E.items():
            if E.seq > 0:
                toks.append(("E", en, E.seq))
        for en in self.E:
            self._wait(en, toks)
        for b in self.bufs.values():
            b.w = None
            b.r = {}
            if b.sem is not None:
                self.sem_pool.setdefault(b.semk, []).append((b.sem, b.semv))
                b.sem = None
        self.bufs = {}

    def finish(self, out_bufs):
        toks = []
        for b in out_bufs:
            toks.append(b.w)
        self._wait("sp", toks)


class T:
    def __init__(self, h, b):
        self.h = h
        self.b = b

    def __getitem__(self, idx):
        return self.h[idx]


class Ctx:
    def __init__(self, nc, es, S, dbg):
        self.nc = nc
        self.es = es
        self.S = S
        self.NT = S // 128
        self.k = Sched(nc, es)
        self.dbg = dbg
        self.dbg_out = {}

    def uname(self, name):
        self.uid = getattr(self, "uid", 0) + 1
        return "%s_%d" % (name, self.uid)

    def sb(self, name, shape, dt, es=None):
        name = self.uname(name)
        h = (es or self.es).enter_context(self.nc.sbuf_tensor(name, list(shape), dt))
        return T(h, self.k.buf(name))

    def ps(self, name, shape, dt=F32, es=None):
        name = self.uname(name)
        h = (es or self.es).enter_context(self.nc.psum_tensor(name, list(shape), dt))
        return T(h, self.k.buf(name))

    def dram(self, name, shape, dt, kind="Internal"):
        h = self.nc.dram_tensor(name, list(shape), dt, kind=kind)
        return T(h.ap(), self.k.buf(name))


def emit_consts(cx):
    k = cx.k
    c = {}
    c["ones_f"] = cx.sb("ones_f", [128, 128], F32)
    k.op("pool", lambda e: e.memset(c["ones_f"][:], 1.0), writes=[c["ones_f"].b])
    c["ones_b"] = cx.sb("ones_b", [128, 128], BF16)
    k.op("pool", lambda e: e.memset(c["ones_b"][:], 1.0), writes=[c["ones_b"].b])
    c["id_f"] = cx.sb("id_f", [128, 128], F32)
    k.op("pool", lambda e: e.affine_select(out=c["id_f"][:], in_=c["ones_f"][:], pattern=[[1, 128]],
                                           compare_op=ALU.is_equal, fill=0.0, base=0, channel_multiplier=-1),
         reads=[c["ones_f"].b], writes=[c["id_f"].b])
    c["id_b"] = cx.sb("id_b", [128, 128], BF16)
    k.op("pool", lambda e: e.tensor_copy(out=c["id_b"][:], in_=c["id_f"][:]),
         reads=[c["id_f"].b], writes=[c["id_b"].b])
    c["tri_ge"] = cx.sb("tri_ge", [128, 128], BF16)
    k.op("pool", lambda e: e.affine_select(out=c["tri_ge"][:], in_=c["ones_b"][:], pattern=[[-1, 128]],
                                           compare_op=ALU.is_ge, fill=0.0, base=0, channel_multiplier=1),
         reads=[c["ones_b"].b], writes=[c["tri_ge"].b])
    c["zeros_b"] = cx.sb("zeros_b", [128, 512], BF16)
    k.op("pool", lambda e: e.memset(c["zeros_b"][:], 0.0), writes=[c["zeros_b"].b])
    c["eps"] = cx.sb("c_eps", [128, 1], F32)
    k.op("pool", lambda e: e.memset(c["eps"][:], EPS), writes=[c["eps"].b])
    c["one"] = cx.sb("c_one", [128, 1], F32)
    k.op("pool", lambda e: e.memset(c["one"][:], 1.0), writes=[c["one"].b])
    return c


def emit_mod(cx, c, L, modb, cT, ada_w, ada_b, norm_mix, norm_ffn):
    k, nc = cx.k, cx.nc
    with contextlib.ExitStack() as es:
        cond = cx.sb("cond", [128, 8], F32, es)
        sig = cx.sb("csig", [128, 8], F32, es)
        condB = cx.sb("condB", [128, 8, 128], F32, es)
        wt = [cx.sb("adaw%d" % i, [128, 8, 512], F32, es) for i in range(2)]
        brow = cx.sb("adab", [1, 6144], F32, es)
        nrow = cx.sb("nrow", [1, 2048], F32, es)
        pm = [cx.ps("pmod%d" % i, [128, 512], F32, es) for i in range(2)]
        k.dma("sp", cond[:], cT, writes=[cond.b])
        k.dma("sp", brow[:], ada_b[L:L + 1, :], writes=[brow.b])
        k.dma("sp", nrow[:, 0:1024], norm_mix[L:L + 1, :], writes=[nrow.b])
        k.dma("sp", nrow[:, 1024:2048], norm_ffn[L:L + 1, :], writes=[nrow.b])
        k.op("act", lambda e: e.activation(out=sig[:], in_=cond[:], func=AF.Sigmoid),
             reads=[cond.b], writes=[sig.b])
        k.op("dve", lambda e: e.tensor_tensor(out=cond[:], in0=cond[:], in1=sig[:], op=ALU.mult),
             reads=[sig.b], writes=[cond.b])
        for kk in range(8):
            k.op("dve", lambda e, kk=kk: e.tensor_scalar(out=condB[:, kk, :], in0=c["ones_f"][:],
                                                        scalar1=cond[:, kk:kk + 1], scalar2=None, op0=ALU.mult),
                 reads=[cond.b, c["ones_f"].b], writes=[condB.b])
        for j in range(12):
            w = wt[j % 2]
            p = pm[j % 2]
            src = ada_w[L, :, j * 512:(j + 1) * 512].rearrange("(k p) f -> p k f", p=128)
            k.dma("sp", w[:], src, writes=[w.b])
            for kk in range(8):
                k.op("pe", lambda e, kk=kk: e.matmul(p[:], lhsT=condB[:, kk, :], rhs=w[:, kk, :],
                                                    start=(kk == 0), stop=False),
                     reads=[condB.b, w.b], writes=[p.b])
            k.op("pe", lambda e: e.matmul(p[:], lhsT=c["ones_f"][0:1, :], rhs=brow[0:1, j * 512:(j + 1) * 512],
                                          start=False, stop=True),
                 reads=[c["ones_f"].b, brow.b], writes=[p.b])
            k.op("act", lambda e: e.activation(out=modb[:, j * 512:(j + 1) * 512], in_=p[:], func=AF.Identity),
                 reads=[p.b], writes=[modb.b])
        for which, col in ((0, 1024), (1, 4096)):
            for hh in range(2):
                p = pm[hh]
                k.op("pe", lambda e: e.matmul(p[:], lhsT=c["ones_f"][0:1, :],
                                              rhs=nrow[0:1, which * 1024 + hh * 512: which * 1024 + (hh + 1) * 512],
                                              start=True, stop=True),
                     reads=[c["ones_f"].b, nrow.b], writes=[p.b])
                sl = modb[:, col + hh * 512: col + (hh + 1) * 512]
                k.op("dve", lambda e: e.scalar_tensor_tensor(out=sl, in0=sl, scalar=1.0, in1=p[:],
                                                             op0=ALU.add, op1=ALU.mult),
                     reads=[p.b], writes=[modb.b])
    k.barrier()


def emit_norm(cx, c, es, src_tile, modb, gcol, scol, dstT, tiles, tok0, router=None, rows_dst=None):
    k = cx.k
    NBUF = 3
    tiles = list(tiles)
    xin = [cx.sb("n_xin%d" % i, [128, D], F32, es) for i in range(NBUF)]
    junk = cx.sb("n_junk", [128, D], BF16, es)
    tmp = [cx.sb("n_tmp%d" % i, [128, D], F32, es) for i in range(NBUF)]
    st = [cx.sb("n_st%d" % i, [128, 4], F32, es) for i in range(NBUF)]
    if rows_dst is None:
        hrow = [cx.sb("n_hrow%d" % i, [128, D], BF16, es) for i in range(NBUF)]
        ptr = [cx.ps("n_ptr%d" % i, [128, 8, 128], BF16, es) for i in range(2)]
    if router is not None:
        hf = [cx.sb("n_hf%d" % i, [128, D], F32, es) for i in range(NBUF)]
        ptf = [[cx.ps("n_ptf", [128, 4, 128], F32, es) for i in range(2)] for half in range(2)]
        hTf = [cx.sb("n_hTf%d" % i, [128, 8, 128], F32, es) for i in range(2)]
        plog = [cx.ps("n_plog%d" % i, [128, 16], F32, es) for i in range(2)]

    def stA(n):
        i = tiles[n]
        x, s = xin[n % NBUF], st[n % NBUF]
        k.dma("sp", x[:], src_tile(i), reads=[src_tile.b], writes=[x.b])
        k.op("act", lambda e: e.activation(out=junk[:], in_=x[:], func=AF.Square, accum_out=s[:, 0:1]),
             reads=[x.b], writes=[junk.b, s.b])
        k.op("pool", lambda e: e.tensor_scalar(out=s[:, 1:2], in0=s[:, 0:1], scalar1=1.0 / D, scalar2=EPS,
                                               op0=ALU.mult, op1=ALU.add), writes=[s.b])
        k.op("act", lambda e: e.activation(out=s[:, 2:3], in_=s[:, 1:2], func=AF.Ln), writes=[s.b])
        k.op("act", lambda e: e.activation(out=s[:, 3:4], in_=s[:, 2:3], func=AF.Exp, scale=-0.5), writes=[s.b])

    def stB(n):
        i = tiles[n]
        x, s, t = xin[n % NBUF], st[n % NBUF], tmp[n % NBUF]
        k.op("dve", lambda e: e.scalar_tensor_tensor(out=t[:], in0=x[:], scalar=s[:, 3:4],
                                                     in1=modb[:, gcol:gcol + D], op0=ALU.mult, op1=ALU.mult),
             reads=[x.b, s.b, modb.b], writes=[t.b])
        if router is None:
            hr = hrow[n % NBUF]
            k.op("dve", lambda e: e.tensor_tensor(out=hr[:], in0=t[:], in1=modb[:, scol:scol + D], op=ALU.add),
                 reads=[t.b, modb.b], writes=[hr.b])
        else:
            h32 = hf[n % NBUF]
            k.op("dve", lambda e: e.tensor_tensor(out=h32[:], in0=t[:], in1=modb[:, scol:scol + D], op=ALU.add),
                 reads=[t.b, modb.b], writes=[h32.b])
            if rows_dst is not None:
                k.op("act", lambda e: e.activation(out=rows_dst[:, i, :], in_=h32[:], func=AF.Identity),
                     reads=[h32.b], writes=[rows_dst.b])
            else:
                hr = hrow[n % NBUF]
                k.op("act", lambda e: e.activation(out=hr[:], in_=h32[:], func=AF.Identity),
                     reads=[h32.b], writes=[hr.b])
        if rows_dst is None:
            p = ptr[n % 2]
            for kk in range(8):
                k.op("pe", lambda e: e.transpose(p[:, kk, :], hr[:, kk * 128:(kk + 1) * 128], c["id_b"][:]),
                     reads=[hr.b, c["id_b"].b], writes=[p.b])
        if router is not None:
            for half in range(2):
                pf = ptf[half][n % 2]
                for kk in range(4):
                    kc = half * 4 + kk
                    k.op("pe", lambda e: e.transpose(pf[:, kk, :], h32[:, kc * 128:(kc + 1) * 128], c["id_f"][:]),
                         reads=[h32.b, c["id_f"].b], writes=[pf.b])

    def stC(n):
        i = tiles[n]
        if rows_dst is None:
            p = ptr[n % 2]
            col = i * 128 - tok0
            k.op("dve", lambda e: e.tensor_copy(out=dstT[:, :, col:col + 128], in_=p[:]),
                 reads=[p.b], writes=[dstT.b])
        if router is not None:
            hT32 = hTf[n % 2]
            for half in range(2):
                pf = ptf[half][n % 2]
                k.op("act", lambda e: e.activation(out=hT32[:, half * 4:(half + 1) * 4, :], in_=pf[:],
                                                   func=AF.Identity), reads=[pf.b], writes=[hT32.b])
            pl = plog[n % 2]
            for kk in range(8):
                k.op("pe", lambda e: e.matmul(pl[:], lhsT=hT32[:, kk, :], rhs=router["rw"][:, kk, :],
                                              start=(kk == 0), stop=(kk == 7)),
                     reads=[hT32.b, router["rw"].b], writes=[pl.b])
            k.op("dve", lambda e: e.tensor_tensor(out=router["logits"][:, i, :], in0=pl[:], in1=router["rb"][:],
                                                  op=ALU.add),
                 reads=[pl.b, router["rb"].b], writes=[router["logits"].b])

    N = len(tiles)
    for step in range(N + 2):
        if step < N:
            stA(step)
        if 0 <= step - 1 < N:
            stB(step - 1)
        if 0 <= step - 2 < N:
            stC(step - 2)


def emit_router(cx, es, logits, comb, t0, t1, G=None, combg=None):
    k = cx.k
    n = t1 - t0
    mx = cx.sb("r_mx", [128, n], F32, es)
    u = cx.sb("r_u", [128, n, 4, 4], F32, es)
    ps_ = cx.sb("r_ps", [128, n, 4, 6], F32, es)
    sel = cx.sb("r_sel", [128, n, 4, 6], F32, es)
    msk = cx.sb("r_msk", [128, n, 4, 4], F32, es)
    rm = cx.sb("r_rm", [128, n], F32, es)
    lg = logits[:, t0:t1, :]
    uf = u[:].rearrange("p n g e -> p n (g e)")
    k.op("dve", lambda e: e.tensor_reduce(out=mx[:], in_=lg, axis=AX.X, op=ALU.max),
         reads=[logits.b], writes=[mx.b])
    k.op("dve", lambda e: e.tensor_tensor(out=uf, in0=lg, in1=mx[:].unsqueeze(2).to_broadcast([128, n, 16]),
                                          op=ALU.subtract),
         reads=[logits.b, mx.b], writes=[u.b])
    k.op("act", lambda e: e.activation(out=uf, in_=uf, func=AF.Exp), reads=[], writes=[u.b])
    k.op("dve", lambda e: e.tensor_tensor(out=ps_[:, :, :, 0:3], in0=u[:, :, :, 0:3], in1=u[:, :, :, 1:4], op=ALU.add),
         reads=[u.b], writes=[ps_.b])
    k.op("dve", lambda e: e.tensor_tensor(out=ps_[:, :, :, 3:5], in0=u[:, :, :, 0:2], in1=u[:, :, :, 2:4], op=ALU.add),
         reads=[u.b], writes=[ps_.b])
    k.op("dve", lambda e: e.tensor_tensor(out=ps_[:, :, :, 5:6], in0=u[:, :, :, 0:1], in1=u[:, :, :, 3:4], op=ALU.add),
         reads=[u.b], writes=[ps_.b])
    psf = ps_[:].rearrange("p n g e -> p n (g e)")
    k.op("dve", lambda e: e.tensor_reduce(out=mx[:], in_=psf, axis=AX.X, op=ALU.max),
         reads=[ps_.b], writes=[mx.b])
    k.op("dve", lambda e: e.tensor_tensor(out=sel[:].rearrange("p n g e -> p n (g e)"), in0=psf,
                                          in1=mx[:].unsqueeze(2).to_broadcast([128, n, 24]), op=ALU.is_ge),
         reads=[ps_.b, mx.b], writes=[sel.b])
    k.op("dve", lambda e: e.reciprocal(out=rm[:], in_=mx[:]), reads=[mx.b], writes=[rm.b])
    k.op("pool", lambda e: e.memset(msk[:], 0.0), writes=[msk.b])
    for (a0, a1, s0, s1) in ((0, 3, 0, 3), (1, 4, 0, 3), (0, 2, 3, 5), (2, 4, 3, 5), (0, 1, 5, 6), (3, 4, 5, 6)):
        k.op("dve", lambda e, a0=a0, a1=a1, s0=s0, s1=s1: e.tensor_tensor(
            out=msk[:, :, :, a0:a1], in0=msk[:, :, :, a0:a1], in1=sel[:, :, :, s0:s1], op=ALU.add),
            reads=[sel.b], writes=[msk.b])
    k.op("dve", lambda e: e.tensor_tensor(out=uf, in0=uf, in1=msk[:].rearrange("p n g e -> p n (g e)"), op=ALU.mult),
         reads=[msk.b], writes=[u.b])
    k.op("dve", lambda e: e.tensor_tensor(out=comb[:, t0:t1, :], in0=uf,
                                          in1=rm[:].unsqueeze(2).to_broadcast([128, n, 16]), op=ALU.mult),
         reads=[u.b, rm.b], writes=[comb.b])
    if G is None:
        return
    gh = cx.sb("r_gh", [128, n, 4], F32, es)
    npv = cx.sb("r_np", [128, n], F32, es)
    k.op("dve", lambda e: e.tensor_reduce(out=gh[:], in_=sel[:], axis=AX.X, op=ALU.max), reads=[sel.b], writes=[gh.b])
    k.op("dve", lambda e: e.tensor_copy(out=G[:, :, 0], in_=gh[:, :, 0]), reads=[gh.b], writes=[G.b])
    k.op("dve", lambda e: e.tensor_scalar(out=npv[:], in0=gh[:, :, 0], scalar1=-1.0, scalar2=1.0, op0=ALU.mult,
                                          op1=ALU.add), reads=[gh.b], writes=[npv.b])
    for g in range(1, 4):
        k.op("dve", lambda e: e.tensor_tensor(out=G[:, :, g], in0=gh[:, :, g], in1=npv[:], op=ALU.mult),
             reads=[gh.b, npv.b], writes=[G.b])
        if g < 3:
            k.op("dve", lambda e: e.tensor_tensor(out=npv[:], in0=npv[:], in1=G[:, :, g], op=ALU.subtract),
                 reads=[G.b], writes=[npv.b])
    prod = cx.sb("r_prod", [128, n, 4, 4], F32, es)
    k.op("dve", lambda e: e.tensor_tensor(out=prod[:], in0=comb[:, t0:t1, :].rearrange("p n (g j) -> p n g j", g=4),
                                          in1=G[:].unsqueeze(3).to_broadcast([128, n, 4, 4]), op=ALU.mult),
         reads=[comb.b, G.b], writes=[prod.b])
    k.op("dve", lambda e: e.tensor_reduce(out=combg[:], in_=prod[:].rearrange("p n g j -> p n j g"), axis=AX.X,
                                          op=ALU.add), reads=[prod.b], writes=[combg.b])


def emit_moe(cx, es, L, h2T, comb, acc, t0, ntile, w_gate, w_up, w_down):
    k = cx.k
    wg = [cx.sb("m_wg%d" % i, [128, 8, DFF], BF16, es) for i in range(2)]
    wu = [cx.sb("m_wu%d" % i, [128, 8, DFF], BF16, es) for i in range(2)]
    wd = [cx.sb("m_wd%d" % i, [128, 4, D], BF16, es) for i in range(2)]
    sg = [cx.sb("m_sg%d" % i, [128, 512], F32, es) for i in range(2)]
    hid = [cx.sb("m_hid%d" % i, [128, 4, 512], BF16, es) for i in range(2)]
    pg = [cx.ps("m_pg%d" % i, [128, 512], F32, es) for i in range(2)]
    pu = [cx.ps("m_pu%d" % i, [128, 512], F32, es) for i in range(2)]
    pd = [cx.ps("m_pd%d" % i, [128, 512], F32, es) for i in range(2)]
    k.op("pool", lambda e: e.memset(acc[:], 0.0), writes=[acc.b])
    ngrp = ntile // 4

    def load(e):
        i = e % 2
        k.dma("pool", wg[i][:], w_gate[L, e].rearrange("(k p) f -> p k f", p=128), writes=[wg[i].b])
        k.dma("pool", wu[i][:], w_up[L, e].rearrange("(k p) f -> p k f", p=128), writes=[wu[i].b])
        k.dma("pool", wd[i][:], w_down[L, e].rearrange("(k p) f -> p k f", p=128), writes=[wd[i].b])

    load(0)
    it = 0
    for e in range(NEXP):
        if e + 1 < NEXP:
            load(e + 1)
        g_, u_, d_ = wg[e % 2], wu[e % 2], wd[e % 2]
        for tg in range(ngrp):
            hd = hid[it % 2]
            it += 1
            c0 = tg * 512
            for fc in range(4):
                pG = pg[fc % 2]
                pU = pu[fc % 2]
                s_ = sg[fc % 2]
                for kk in range(8):
                    k.op("pe", lambda en, kk=kk: en.matmul(pG[:], lhsT=g_[:, kk, fc * 128:(fc + 1) * 128],
                                                          rhs=h2T[:, kk, c0:c0 + 512], start=(kk == 0), stop=(kk == 7)),
                         reads=[g_.b, h2T.b], writes=[pG.b])
                for kk in range(8):
                    k.op("pe", lambda en, kk=kk: en.matmul(pU[:], lhsT=u_[:, kk, fc * 128:(fc + 1) * 128],
                                                          rhs=h2T[:, kk, c0:c0 + 512], start=(kk == 0), stop=(kk == 7)),
                         reads=[u_.b, h2T.b], writes=[pU.b])
                k.op("act", lambda en: en.activation(out=s_[:], in_=pG[:], func=AF.Silu),
                     reads=[pG.b], writes=[s_.b])
                k.op("dve", lambda en: en.tensor_tensor(out=hd[:, fc, :], in0=pU[:], in1=s_[:], op=ALU.mult),
                     reads=[pU.b, s_.b], writes=[hd.b])
            for tt in range(4):
                ti = tg * 4 + tt
                for dh in range(2):
                    pD = pd[dh]
                    for fc in range(4):
                        k.op("pe", lambda en, fc=fc: en.matmul(pD[:], lhsT=hd[:, fc, tt * 128:(tt + 1) * 128],
                                                              rhs=d_[:, fc, dh * 512:(dh + 1) * 512],
                                                              start=(fc == 0), stop=(fc == 3)),
                             reads=[hd.b, d_.b], writes=[pD.b])
                    a = acc[:, ti, dh * 512:(dh + 1) * 512]
                    k.op("dve", lambda en: en.scalar_tensor_tensor(out=a, in0=pD[:], scalar=comb[:, t0 + ti, e:e + 1],
                                                                   in1=a, op0=ALU.mult, op1=ALU.add),
                         reads=[pD.b, comb.b], writes=[acc.b])


def emit_ffn(cx, c, L, modb, xsrc, xdst, router_w, router_b, w_gate, w_up, w_down, nhalf=2):
    k = cx.k
    NT = cx.NT
    per = NT // nhalf
    with contextlib.ExitStack() as es:
        rw = cx.sb("f_rw", [128, 8, 16], F32, es)
        rb = cx.sb("f_rb", [128, 16], F32, es)
        rb1 = cx.sb("f_rb1", [1, 16], F32, es)
        logits = cx.sb("f_logits", [128, NT, 16], F32, es)
        comb = cx.sb("f_comb", [128, NT, 16], F32, es)
        h2T = cx.sb("f_h2T", [128, 8, per * 128], BF16, es)
        acc = cx.sb("f_acc", [128, per, D], F32, es)
        k.dma("sp", rw[:], router_w.rearrange("(k p) e -> p k e", p=128), writes=[rw.b])
        k.dma("sp", rb1[:], router_b, writes=[rb1.b])
        with contextlib.ExitStack() as es2:
            prb = cx.ps("f_prb", [128, 16], F32, es2)
            k.op("pe", lambda e: e.matmul(prb[:], lhsT=c["ones_f"][0:1, :], rhs=rb1[0:1, :], start=True, stop=True),
                 reads=[c["ones_f"].b, rb1.b], writes=[prb.b])
            k.op("act", lambda e: e.activation(out=rb[:], in_=prb[:], func=AF.Identity), reads=[prb.b], writes=[rb.b])
            k.barrier()
        for half in range(nhalf):
            t0 = half * per
            with contextlib.ExitStack() as es2:
                emit_norm(cx, c, es2, xsrc, modb, 4096, 3072, h2T, range(t0, t0 + per), t0 * 128,
                          router=dict(rw=rw, rb=rb, logits=logits))
                emit_router(cx, es2, logits, comb, t0, t0 + per)
                k.barrier()
            with contextlib.ExitStack() as es2:
                emit_moe(cx, es2, L, h2T, comb, acc, t0, per, w_gate, w_up, w_down)
                k.barrier()
            with contextlib.ExitStack() as es2:
                xin = [cx.sb("o_xin%d" % i, [128, D], F32, es2) for i in range(2)]
                xo = [cx.sb("o_xo%d" % i, [128, D], F32, es2) for i in range(2)]
                for n in range(per):
                    i = t0 + n
                    x = xin[n % 2]
                    o = xo[n % 2]
                    k.dma("sp", x[:], xsrc(i), reads=[xsrc.b], writes=[x.b])
                    k.op("dve", lambda e: e.tensor_tensor(out=o[:], in0=acc[:, n, :], in1=modb[:, 5120:6144],
                                                          op=ALU.mult), reads=[acc.b, modb.b], writes=[o.b])
                    k.op("pool", lambda e: e.tensor_tensor(out=o[:], in0=o[:], in1=x[:], op=ALU.add),
                         reads=[x.b], writes=[o.b])
                    k.dma("sp", xdst(i), o[:], reads=[o.b], writes=[xdst.b])
                k.barrier()


def load_w(cx, wt, w_in, L, c0, ncol):
    cx.k.dma("pool", wt[:, :, 0:ncol], w_in[L, :, c0:c0 + ncol].rearrange("(k p) c -> p k c", p=128),
             writes=[wt.b])


def proj_featT(cx, hT, wt, m, pbanks, evac):
    k = cx.k
    for tg in range(cx.S // 512):
        p = pbanks[tg % len(pbanks)]
        for kk in range(8):
            k.op("pe", lambda e: e.matmul(p[0:m, :], lhsT=wt[:, kk, 0:m], rhs=hT[:, kk, tg * 512:(tg + 1) * 512],
                                          start=(kk == 0), stop=(kk == 7)),
                 reads=[wt.b, hT.b], writes=[p.b])
        evac(tg, p)


class QKNorm:
    def __init__(self, cx, c, es):
        self.cx, self.c = cx, c
        self.sq = [cx.sb("qn_sq%d" % i, [64, 512], BF16, es) for i in range(2)]
        self.r = [cx.sb("qn_r%d" % i, [64, 512], F32, es) for i in range(2)]
        self.pss = [cx.ps("qn_pss%d" % i, [64, 512], F32, es) for i in range(2)]
        self.n = 0

    def __call__(self, p, gain, dst_ap, dst_buf):
        k, c = self.cx.k, self.c
        sq, r, pss = self.sq[self.n % 2], self.r[self.n % 2], self.pss[self.n % 2]
        self.n += 1
        k.op("act", lambda e: e.activation(out=sq[:], in_=p[0:64, :], func=AF.Square), reads=[p.b], writes=[sq.b])
        k.op("pe", lambda e: e.matmul(pss[:], lhsT=c["ones_b"][0:64, 0:64], rhs=sq[:], start=True, stop=True),
             reads=[sq.b, c["ones_b"].b], writes=[pss.b])
        k.op("act", lambda e: e.activation(out=r[:], in_=pss[:], func=AF.Ln, scale=1.0 / HD, bias=c["eps"][0:64, :]),
             reads=[pss.b, c["eps"].b], writes=[r.b])
        k.op("act", lambda e: e.activation(out=r[:], in_=r[:], func=AF.Exp, scale=-0.5), writes=[r.b])
        k.op("dve", lambda e: e.scalar_tensor_tensor(out=dst_ap, in0=p[0:64, :], scalar=gain[:], in1=r[:],
                                                     op0=ALU.mult, op1=ALU.mult),
             reads=[p.b, r.b, gain.b], writes=[dst_buf])


def load_gain(cx, es, name, src, L, scale):
    k = cx.k
    g = cx.sb(name, [64, 1], F32, es)
    k.dma("sp", g[:], src[L:L + 1, :].rearrange("o e -> e o"), writes=[g.b])
    if scale != 1.0:
        k.op("dve", lambda e: e.tensor_scalar(out=g[:], in0=g[:], scalar1=scale, scalar2=None, op0=ALU.mult),
             writes=[g.b])
    return g


def emit_fox(cx, c, L, hT, w_in, b_forget, q_gain, k_gain, mixedT):
    k = cx.k
    S, NT, NG = cx.S, cx.NT, cx.S // 512
    with contextlib.ExitStack() as es:
        vaug = cx.sb("fx_vaug", [128, NT, 4, 65], BF16, es)
        cs3 = cx.sb("fx_cs3", [4, 3, S], BF16, es)
        qa = [cx.sb("fx_qa%d" % i, [70, S], BF16, es) for i in range(2)]
        ka = [cx.sb("fx_ka%d" % i, [70, S], BF16, es) for i in range(2)]
        gq = load_gain(cx, es, "fx_gq", q_gain, L, 0.125)
        gk = load_gain(cx, es, "fx_gk", k_gain, L, 1.0)
        with contextlib.ExitStack() as es2:
            wv = cx.sb("fx_wv", [128, 8, 256], BF16, es2)
            wf = cx.sb("fx_wf", [128, 8, 4], BF16, es2)
            nb = cx.sb("fx_nb", [4, 1], F32, es2)
            A = cx.sb("fx_A", [4, S], F32, es2)
            B = cx.sb("fx_B", [4, S], F32, es2)
            pv = [cx.ps("fx_pv%d" % i, [128, 512], F32, es2) for i in range(2)]
            pf = [cx.ps("fx_pf%d" % i, [128, 512], F32, es2) for i in range(2)]
            load_w(cx, wv, w_in, L, 512, 256)
            load_w(cx, wf, w_in, L, 1024, 4)
            k.dma("sp", nb[:], b_forget[L:L + 1, :].rearrange("o h -> h o"), writes=[nb.b])
            k.op("dve", lambda e: e.tensor_scalar(out=nb[:], in0=nb[:], scalar1=-1.0, scalar2=None, op0=ALU.mult),
                 writes=[nb.b])
            k.op("pool", lambda e: e.memset(vaug[:], 1.0), writes=[vaug.b])
            for i in range(NT):
                p = pv[i % 2]
                for kk in range(8):
                    k.op("pe", lambda e: e.matmul(p[:, 0:256], lhsT=hT[:, kk, i * 128:(i + 1) * 128], rhs=wv[:, kk, :],
                                                  start=(kk == 0), stop=(kk == 7)),
                         reads=[hT.b, wv.b], writes=[p.b])
                k.op("act", lambda e: e.activation(out=vaug[:, i, :, 0:64],
                                                   in_=p[:, 0:256].rearrange("p (h e) -> p h e", h=4),
                                                   func=AF.Identity), reads=[p.b], writes=[vaug.b])
            for tg in range(NG):
                p = pf[tg % 2]
                for kk in range(8):
                    k.op("pe", lambda e: e.matmul(p[0:4, :], lhsT=wf[:, kk, :], rhs=hT[:, kk, tg * 512:(tg + 1) * 512],
                                                  start=(kk == 0), stop=(kk == 7)),
                         reads=[hT.b, wf.b], writes=[p.b])
                k.op("act", lambda e: e.activation(out=A[:, tg * 512:(tg + 1) * 512], in_=p[0:4, :], func=AF.Exp,
                                                   scale=-1.0, bias=nb[:]), reads=[p.b, nb.b], writes=[A.b])
            k.op("act", lambda e: e.activation(out=A[:], in_=A[:], func=AF.Ln, bias=c["one"][0:4, :]),
                 reads=[c["one"].b], writes=[A.b])
            k.op("dve", lambda e: e.tensor_tensor_scan(out=B[:], data0=A[:], data1=A[:], initial=0.0,
                                                       op0=ALU.add, op1=ALU.max), reads=[A.b], writes=[B.b])
            k.op("dve", lambda e: e.tensor_copy(out=cs3[:, 0, :], in_=B[:]), reads=[B.b], writes=[cs3.b])
            k.op("dve", lambda e: e.tensor_tensor(out=A[:], in0=B[:], in1=cs3[:, 0, :], op=ALU.subtract),
                 reads=[B.b, cs3.b], writes=[A.b])
            k.op("dve", lambda e: e.tensor_copy(out=cs3[:, 1, :], in_=A[:]), reads=[A.b], writes=[cs3.b])
            k.op("dve", lambda e: e.tensor_tensor(out=A[:], in0=A[:], in1=cs3[:, 1, :], op=ALU.subtract),
                 reads=[cs3.b], writes=[A.b])
            k.op("dve", lambda e: e.tensor_copy(out=cs3[:, 2, :], in_=A[:]), reads=[A.b], writes=[cs3.b])
            k.barrier()
        with contextlib.ExitStack() as es2:
            wq = [cx.sb("fx_wq%d" % i, [128, 8, 64], BF16, es2) for i in range(2)]
            wk = [cx.sb("fx_wk%d" % i, [128, 8, 64], BF16, es2) for i in range(2)]
            wg = [cx.sb("fx_wg%d" % i, [128, 8, 64], BF16, es2) for i in range(2)]
            lanes = []
            for ln in range(2):
                lanes.append(dict(
                    pt=[cx.sb("fx_pt", [128, 512], BF16, es2) for i in range(3)],
                    rl=cx.sb("fx_rl", [128, 512], F32, es2),
                    osb=cx.sb("fx_osb", [64, 512], F32, es2),
                    eg=cx.sb("fx_eg", [64, 512], F32, es2),
                    ob=[cx.sb("fx_ob", [64, 512], BF16, es2) for i in range(2)],
                    ps=[cx.ps("fx_ps", [128, 512], F32, es2) for i in range(2)],
                    po=cx.ps("fx_po", [128, 512], F32, es2),
                    pm=cx.ps("fx_pm", [128, 512], F32, es2)))
            sq = [cx.sb("qn_sq", [64, 512], BF16, es2) for i in range(2)]
            rr = [cx.sb("qn_r", [64, 512], F32, es2) for i in range(2)]

            def project(h, QA, KA, Wq, Wk, R):
                n = 0
                for (W, dst, gain) in ((Wq, QA, gq), (Wk, KA, gk)):
                    for tg in range(NG):
                        p, pss = R["ps"][tg % 2], (R["po"], R["pm"])[tg % 2]
                        s_, r_ = sq[n % 2], rr[n % 2]
                        n += 1
                        for kk in range(8):
                            k.op("pe", lambda e: e.matmul(p[0:64, :], lhsT=W[:, kk, :],
                                                          rhs=hT[:, kk, tg * 512:(tg + 1) * 512],
                                                          start=(kk == 0), stop=(kk == 7)),
                                 reads=[W.b, hT.b], writes=[p.b])
                        k.op("act", lambda e: e.activation(out=s_[:], in_=p[0:64, :], func=AF.Square),
                             reads=[p.b], writes=[s_.b])
                        k.op("pe", lambda e: e.matmul(pss[0:64, :], lhsT=c["ones_b"][0:64, 0:64], rhs=s_[:],
                                                      start=True, stop=True), reads=[s_.b, c["ones_b"].b],
                             writes=[pss.b])
                        k.op("act", lambda e: e.activation(out=r_[:], in_=pss[0:64, :], func=AF.Ln, scale=1.0 / HD,
                                                           bias=c["eps"][0:64, :]), reads=[pss.b, c["eps"].b],
                             writes=[r_.b])
                        k.op("act", lambda e: e.activation(out=r_[:], in_=r_[:], func=AF.Exp, scale=-0.5),
                             writes=[r_.b])
                        k.op("dve", lambda e: e.scalar_tensor_tensor(out=dst[0:64, tg * 512:(tg + 1) * 512],
                                                                     in0=p[0:64, :], scalar=gain[:], in1=r_[:],
                                                                     op0=ALU.mult, op1=ALU.mult),
                             reads=[p.b, r_.b, gain.b], writes=[dst.b])

            def attn_gen(h, QA, KA, Wg, R):
                pt, rl, osb, eg, pO, pG = R["pt"], R["rl"], R["osb"], R["eg"], R["po"], R["pm"]
                for g in range(NG):
                    c0 = g * 512
                    nkb = 4 * g + 4
                    steps = [(kb, 128 * max(kb - 4 * g, 0), kb - 4 * g >= 0) for kb in range(nkb)]

                    def stA(j):
                        kb, lo, dg = steps[j]
                        pS = R["ps"][j % 2]
                        k.op("pe", lambda e: e.matmul(pS[:, lo:512], lhsT=KA[0:70, kb * 128:(kb + 1) * 128],
                                                      rhs=QA[0:70, c0 + lo:c0 + 512], start=True, stop=True),
                             reads=[KA.b, QA.b], writes=[pS.b])

                    def stB(j):
                        kb, lo, dg = steps[j]
                        pS, P = R["ps"][j % 2], pt[j % 3]
                        k.op("act", lambda e: e.activation(out=P[:, lo:512], in_=pS[:, lo:512], func=AF.Exp),
                             reads=[pS.b], writes=[P.b])
                        if dg:
                            k.op("pool", lambda e: e.affine_select(out=P[:, lo:lo + 128], in_=P[:, lo:lo + 128],
                                                                   pattern=[[1, 128]], compare_op=ALU.is_ge, fill=0.0,
                                                                   base=0, channel_multiplier=-1), writes=[P.b])

                    def stC(j):
                        kb, lo, dg = steps[j]
                        P = pt[j % 3]
                        k.op("pe", lambda e: e.matmul(pO[0:65, lo:512], lhsT=vaug[:, kb, h, :], rhs=P[:, lo:512],
                                                      start=(j == 0), stop=(j == nkb - 1)),
                             reads=[vaug.b, P.b], writes=[pO.b])

                    stA(0)
                    stA(1)
                    for j in range(nkb):
                        stB(j)
                        stC(j)
                        if j + 2 < nkb:
                            stA(j + 2)
                        if j % 2 == 1:
                            yield
                    O = R["ob"][g % 2]
                    k.op("act", lambda e: e.activation(out=rl[64:65, :], in_=pO[64:65, :], func=AF.Ln),
                         reads=[pO.b], writes=[rl.b])
                    k.op("act", lambda e: e.activation(out=rl[64:65, :], in_=rl[64:65, :], func=AF.Exp, scale=-1.0),
                         writes=[rl.b])
                    k.op("act", lambda e: e.activation(out=osb[:], in_=pO[0:64, :], func=AF.Identity),
                         reads=[pO.b], writes=[osb.b])
                    pB = R["ps"][0]
                    k.op("pe", lambda e: e.matmul(pB[0:64, :], lhsT=c["ones_f"][64:65, 0:64], rhs=rl[64:65, :],
                                                  start=True, stop=True), reads=[rl.b, c["ones_f"].b], writes=[pB.b])
                    for kk in range(8):
                        k.op("pe", lambda e: e.matmul(pG[0:64, :], lhsT=Wg[:, kk, :], rhs=hT[:, kk, c0:c0 + 512],
                                                      start=(kk == 0), stop=(kk == 7)),
                             reads=[Wg.b, hT.b], writes=[pG.b])
                    k.op("act", lambda e: e.activation(out=eg[:], in_=pG[0:64, :], func=AF.Exp, scale=-1.0),
                         reads=[pG.b], writes=[eg.b])
                    k.op("act", lambda e: e.activation(out=eg[:], in_=eg[:], func=AF.Ln, bias=c["one"][0:64, :]),
                         reads=[c["one"].b], writes=[eg.b])
                    k.op("act", lambda e: e.activation(out=eg[:], in_=eg[:], func=AF.Exp, scale=-1.0), writes=[eg.b])
                    k.op("dve", lambda e: e.tensor_tensor(out=osb[:], in0=osb[:], in1=pB[0:64, :], op=ALU.mult),
                         reads=[pB.b], writes=[osb.b])
                    k.op("dve", lambda e: e.tensor_tensor(out=O[:], in0=osb[:], in1=eg[:], op=ALU.mult),
                         reads=[osb.b, eg.b], writes=[O.b])
                    k.dma("sp", mixedT[64 * h:64 * h + 64, c0:c0 + 512], O[:], reads=[O.b], writes=[mixedT.b])
                    yield

            for pr_ in range(2):
                gens = []
                for ln in range(2):
                    h = 2 * pr_ + ln
                    QA, KA = qa[ln], ka[ln]
                    load_w(cx, wq[ln], w_in, L, 64 * h, 64)
                    load_w(cx, wk[ln], w_in, L, 256 + 64 * h, 64)
                    load_w(cx, wg[ln], w_in, L, 768 + 64 * h, 64)
                    k.op("pool", lambda e: e.memset(QA[64:70, :], 1.0), writes=[QA.b])
                    k.op("pool", lambda e: e.memset(KA[64:70, :], 1.0), writes=[KA.b])
                    for j in range(3):
                        k.dma("sp", QA[64 + j:65 + j, :], cs3[h:h + 1, j, :], reads=[cs3.b], writes=[QA.b])
                        k.dma("sp", KA[67 + j:68 + j, :], cs3[h:h + 1, j, :], reads=[cs3.b], writes=[KA.b])
                    k.op("dve", lambda e: e.tensor_scalar(out=QA[64:67, :], in0=QA[64:67, :], scalar1=-1.0,
                                                          scalar2=None, op0=ALU.mult), writes=[QA.b])
                    project(h, QA, KA, wq[ln], wk[ln], lanes[ln])
                    gens.append(attn_gen(h, QA, KA, wg[ln], lanes[ln]))
                run_lanes(gens)
            k.barrier()


def run_lanes(gens):
    gens = list(gens)
    while gens:
        for g in list(gens):
            try:
                next(g)
            except StopIteration:
                gens.remove(g)


def emit_sb(cx, c, L, hT, w_in, mixedT):
    k = cx.k
    S, NT, NG = cx.S, cx.NT, cx.S // 512
    QOFF, KOFF, VOFF, MOFF = 2180, 2564, 2948, 640
    NL = 2
    with contextlib.ExitStack() as es:
        vs = cx.sb("sb_v", [128, NT, 6, 64], BF16, es)
        with contextlib.ExitStack() as es2:
            wv = cx.sb("sb_wv", [128, 8, 384], BF16, es2)
            pv = [cx.ps("sb_pv%d" % i, [128, 512], F32, es2) for i in range(2)]
            load_w(cx, wv, w_in, L, VOFF, 384)
            for i in range(NT):
                p = pv[i % 2]
                for kk in range(8):
                    k.op("pe", lambda e: e.matmul(p[:, 0:384], lhsT=hT[:, kk, i * 128:(i + 1) * 128], rhs=wv[:, kk, :],
                                                  start=(kk == 0), stop=(kk == 7)),
                         reads=[hT.b, wv.b], writes=[p.b])
                k.op("act", lambda e: e.activation(out=vs[:, i, :, :],
                                                   in_=p[:, 0:384].rearrange("p (h e) -> p h e", h=6),
                                                   func=AF.Identity), reads=[p.b], writes=[vs.b])
            k.barrier()
        with contextlib.ExitStack() as es2:
            CH = 16
            wq = cx.sb("sb_wq", [128, 8, 128], BF16, es2)
            wk = cx.sb("sb_wk", [128, 8, 128], BF16, es2)
            Kp = cx.sb("sb_kp", [128, S], BF16, es2)
            NKp = cx.sb("sb_nkp", [128, S], BF16, es2)
            Qp = [cx.sb("sb_q", [128, S], BF16, es2) for i in range(2)]
            k.op("pool", lambda e: e.memset(Qp[0][64:128, :], 0.0), writes=[Qp[0].b])
            k.op("pool", lambda e: e.memset(Qp[1][0:64, :], 0.0), writes=[Qp[1].b])
            lanes = []
            for ln in range(NL):
                R = dict(
                    SP=[cx.sb("sb_SP", [128, 512], BF16, es2) for i in range(CH)],
                    A=[cx.sb("sb_A", [128, 512], BF16, es2) for i in range(2)],
                    acc=[cx.sb("sb_acc", [128, 512], BF16, es2) for i in range(2)],
                    ob=[cx.sb("sb_ob", [128, 512], BF16, es2) for i in range(2)],
                    pp=[cx.ps("sb_pp", [128, 512], F32, es2) for i in range(2)],
                    po=cx.ps("sb_po", [128, 512], F32, es2))
                lanes.append(R)

            def head_gen(R, h, Q, half):
                pO = R["po"]
                hs = slice(64 * half, 64 * half + 64)
                Vp = lambda kb: vs[:, kb, 2 * (h // 2):2 * (h // 2) + 2, :].rearrange("p h e -> p (h e)")
                steps = []
                for g in range(NG):
                    nkb = 4 * g + 4
                    for n_, kb in enumerate(range(nkb - 1, -1, -1)):
                        steps.append((g, kb, 128 * max(kb - 4 * g, 0), kb - 4 * g >= 0, n_ == 0, n_ == nkb - 1))
                ns = len(steps)

                def stZ(j):
                    g, kb, lo, dg, first, last = steps[j]
                    c0 = g * 512
                    pZ = R["pp"][j % 2]
                    k.op("pe", lambda e: e.matmul(pZ[:, lo:512], lhsT=Kp[:, kb * 128:(kb + 1) * 128],
                                                  rhs=Q[:, c0 + lo:c0 + 512], start=True, stop=True),
                         reads=[Kp.b, Q.b], writes=[pZ.b])

                def stS(j):
                    g, kb, lo, dg, first, last = steps[j]
                    pZ, SP, AC = R["pp"][j % 2], R["SP"][j % CH], R["acc"][g % 2]
                    if cx.dbg.get("nosoftplus"):
                        k.op("act", lambda e: e.activation(out=SP[:, lo:512], in_=pZ[:, lo:512], func=AF.Exp),
                             reads=[pZ.b], writes=[SP.b])
                        k.op("act", lambda e: e.activation(out=SP[:, lo:512], in_=SP[:, lo:512], func=AF.Ln,
                                                           bias=c["one"][:]), reads=[c["one"].b], writes=[SP.b])
                    else:
                        k.op("act", lambda e: e.activation(out=SP[:, lo:512], in_=pZ[:, lo:512], func=AF.Softplus),
                             reads=[pZ.b], writes=[SP.b])
                    if dg:
                        k.op("pool", lambda e: e.affine_select(out=SP[:, lo:lo + 128], in_=SP[:, lo:lo + 128],
                                                               pattern=[[1, 128]], compare_op=ALU.is_gt, fill=0.0,
                                                               base=0, channel_multiplier=-1), writes=[SP.b])

                def stC(j):
                    g, kb, lo, dg, first, last = steps[j]
                    c0 = g * 512
                    pR, SP, AC = R["pp"][j % 2], R["SP"][j % CH], R["acc"][g % 2]
                    k.op("pe", lambda e: e.matmul(pR[:, lo:512], lhsT=NKp[:, kb * 128:(kb + 1) * 128],
                                                  rhs=Q[:, c0 + lo:c0 + 512], start=True, stop=False),
                         reads=[NKp.b, Q.b], writes=[pR.b])
                    k.op("pe", lambda e: e.matmul(pR[:, lo:512], lhsT=c["tri_ge"][:], rhs=SP[:, lo:512],
                                                  start=False, stop=first), reads=[SP.b, c["tri_ge"].b],
                         writes=[pR.b])
                    if not first:
                        k.op("pe", lambda e: e.matmul(pR[:, lo:512], lhsT=c["ones_b"][:], rhs=AC[:, lo:512],
                                                      start=False, stop=True), reads=[AC.b, c["ones_b"].b],
                             writes=[pR.b])
                    if first:
                        k.op("pool", lambda e: e.memset(AC[:], 0.0), writes=[AC.b])
                    if not last:
                        k.op("dve", lambda e: e.tensor_tensor(out=AC[:, lo:512], in0=AC[:, lo:512],
                                                              in1=SP[:, lo:512], op=ALU.add),
                             reads=[SP.b], writes=[AC.b])

                def stD(j):
                    g, kb, lo, dg, first, last = steps[j]
                    pR, A_ = R["pp"][j % 2], R["A"][j % 2]
                    k.op("act", lambda e: e.activation(out=A_[:, lo:512], in_=pR[:, lo:512], func=AF.Exp,
                                                       scale=-1.0), reads=[pR.b], writes=[A_.b])
                    if dg:
                        k.op("pool", lambda e: e.affine_select(out=A_[:, lo:lo + 128], in_=A_[:, lo:lo + 128],
                                                               pattern=[[1, 128]], compare_op=ALU.is_gt, fill=0.0,
                                                               base=0, channel_multiplier=-1), writes=[A_.b])

                def stE(j):
                    g, kb, lo, dg, first, last = steps[j]
                    A_ = R["A"][j % 2]
                    if first:
                        k.op("pe", lambda e: e.matmul(pO[:, :], lhsT=c["zeros_b"][:, 0:128], rhs=c["zeros_b"][:],
                                                      start=True, stop=False), reads=[c["zeros_b"].b],
                             writes=[pO.b])
                    k.op("pe", lambda e: e.matmul(pO[:, lo:512], lhsT=Vp(kb), rhs=A_[:, lo:512],
                                                  start=False, stop=last), reads=[vs.b, A_.b], writes=[pO.b])
                    if last:
                        O = R["ob"][g % 2]
                        k.op("dve", lambda e: e.tensor_copy(out=O[hs, :], in_=pO[hs, :]),
                             reads=[pO.b], writes=[O.b])
                        k.dma("sp", mixedT[MOFF + 64 * h:MOFF + 64 * h + 64, g * 512:(g + 1) * 512], O[hs, :],
                              reads=[O.b], writes=[mixedT.b])

                for j0 in range(0, ns, CH):
                    j1 = min(ns, j0 + CH)
                    stZ(j0)
                    for j in range(j0, j1):
                        if j + 1 < j1:
                            stZ(j + 1)
                        stS(j)
                        yield
                    stC(j0)
                    for j in range(j0, j1):
                        stD(j)
                        if j + 1 < j1:
                            stC(j + 1)
                        stE(j)
                        yield

            for pr_ in range(3):
                load_w(cx, wq, w_in, L, QOFF + 128 * pr_, 128)
                load_w(cx, wk, w_in, L, KOFF + 128 * pr_, 128)
                for tg in range(NG):
                    cs = slice(tg * 512, (tg + 1) * 512)
                    p = lanes[0]["pp"][tg % 2]
                    for kk in range(8):
                        k.op("pe", lambda e: e.matmul(p[:], lhsT=wq[:, kk, :], rhs=hT[:, kk, cs],
                                                      start=(kk == 0), stop=(kk == 7)),
                             reads=[wq.b, hT.b], writes=[p.b])
                    k.op("act", lambda e: e.activation(out=Qp[0][0:64, cs], in_=p[0:64, :], func=AF.Identity,
                                                       scale=0.125), reads=[p.b], writes=[Qp[0].b])
                    k.op("dve", lambda e: e.tensor_scalar(out=Qp[1][64:128, cs], in0=p[64:128, :], scalar1=0.125,
                                                          scalar2=None, op0=ALU.mult), reads=[p.b], writes=[Qp[1].b])
                    p = lanes[1]["pp"][tg % 2]
                    for kk in range(8):
                        k.op("pe", lambda e: e.matmul(p[:], lhsT=wk[:, kk, :], rhs=hT[:, kk, cs],
                                                      start=(kk == 0), stop=(kk == 7)),
                             reads=[wk.b, hT.b], writes=[p.b])
                    k.op("act", lambda e: e.activation(out=Kp[:, cs], in_=p[:], func=AF.Identity),
                         reads=[p.b], writes=[Kp.b])
                    k.op("act", lambda e: e.activation(out=NKp[:, cs], in_=p[:], func=AF.Identity, scale=-1.0),
                         reads=[p.b], writes=[NKp.b])
                run_lanes([head_gen(lanes[ln], 2 * pr_ + ln, Qp[ln], ln) for ln in range(NL)])
            k.barrier()


DIL_PATTERNS = ((128, 1), (512, 4), (2048, 16))


def emit_dil(cx, c, L, hT, w_in, q_gain, k_gain, tb_gather, tb_mask, mixedT):
    k = cx.k
    S, NT, NG = cx.S, cx.NT, cx.S // 512
    QOFF, KOFF, VOFF, MOFF = 1028, 1412, 1796, 256
    with contextlib.ExitStack() as es:
        tbias = cx.sb("tbias", [128, 6, 3, 2, 128], BF16, es)
        with contextlib.ExitStack() as es2:
            tg32 = cx.sb("tg32", [128, 6, 768], F32, es2)
            tm32 = cx.sb("tm32", [128, 768], F32, es2)
            k.dma("sp", tg32[:], tb_gather, writes=[tg32.b])
            k.dma("sp", tm32[:], tb_mask, writes=[tm32.b])
            for h in range(6):
                k.op("dve", lambda e: e.tensor_tensor(out=tbias[:, h].rearrange("p a b q -> p (a b q)"),
                                                      in0=tg32[:, h, :], in1=tm32[:], op=ALU.add),
                     reads=[tg32.b, tm32.b], writes=[tbias.b])
            k.barrier()
        gq = cx.sb("dl_gq", [128, 1], F32, es)
        gk = cx.sb("dl_gk", [128, 1], F32, es)
        for half in range(2):
            k.dma("sp", gq[64 * half:64 * half + 64, :], q_gain[L:L + 1, :].rearrange("o e -> e o"), writes=[gq.b])
            k.dma("sp", gk[64 * half:64 * half + 64, :], k_gain[L:L + 1, :].rearrange("o e -> e o"), writes=[gk.b])
        k.op("dve", lambda e: e.tensor_scalar(out=gq[:], in0=gq[:], scalar1=0.125, scalar2=None, op0=ALU.mult),
             writes=[gq.b])
        bd = cx.sb("dl_bd", [128, 128], BF16, es)
        k.op("pool", lambda e: e.memset(bd[:], 0.0), writes=[bd.b])
        k.op("pool", lambda e: e.memset(bd[0:64, 0:64], 1.0), writes=[bd.b])
        k.op("pool", lambda e: e.memset(bd[64:128, 64:128], 1.0), writes=[bd.b])
        wq = cx.sb("dl_wq", [128, 8, 128], BF16, es)
        wk = cx.sb("dl_wk", [128, 8, 128], BF16, es)
        wv = cx.sb("dl_wv", [128, 8, 128], BF16, es)
        Kp = cx.sb("dl_kp", [128, S], BF16, es)
        Qp = [cx.sb("dl_q", [128, S], BF16, es) for i in range(2)]
        k.op("pool", lambda e: e.memset(Qp[0][64:128, :], 0.0), writes=[Qp[0].b])
        k.op("pool", lambda e: e.memset(Qp[1][0:64, :], 0.0), writes=[Qp[1].b])
        V = cx.sb("dl_v", [128, 3, NT, 2, 65], BF16, es)
        k.op("pool", lambda e: e.memset(V[:], 1.0), writes=[V.b])
        sq = [cx.sb("dl_sq", [128, 512], BF16, es) for i in range(2)]
        rr = [cx.sb("dl_r", [128, 512], F32, es) for i in range(2)]
        lanes = []
        for ln in range(2):
            lanes.append(dict(
                accs=cx.sb("dl_acc", [65, S], F32, es),
                pt=[cx.sb("dl_pt", [128, 512], BF16, es) for i in range(2)],
                rl=cx.sb("dl_rl", [128, 512], F32, es),
                ob=[cx.sb("dl_ob", [64, 512], BF16, es) for i in range(2)],
                pq=[cx.ps("dl_pq", [128, 512], F32, es) for i in range(2)],
                po=[cx.ps("dl_po", [128, 512], F32, es) for i in range(2)]))

        def attn_gen(h, ln, Q, R):
            accs, pt, rl, pq, po = R["accs"], R["pt"], R["rl"], R["pq"], R["po"]
            steps = []
            nst = 0
            for pi, (win, d) in enumerate(DIL_PATTERNS):
                nb = S // (128 * d)
                per = min(4, nb)
                for r in range(d):
                    for n0 in range(0, nb, per):
                        n2s = list(range(n0, n0 + per, 2))
                        for n2 in n2s:
                            steps.append((pi, d, nb, r, n0, per, n2, min(2, n0 + per - n2), nst, n2 == n2s[-1]))
                        nst += 1

            def stS(j):
                pi, d, nb, r, n0, per, n2, nu, bk, lastb = steps[j]
                pS = pq[j % 2]
                for u in range(nu):
                    n = n2 + u
                    qs = slice(128 * n * d + r, 128 * n * d + r + 127 * d + 1, d)
                    if n > 0:
                        ks = slice(128 * (n - 1) * d + r, 128 * (n - 1) * d + r + 127 * d + 1, d)
                        k.op("pe", lambda e: e.matmul(pS[:, u * 256:u * 256 + 128], lhsT=Kp[:, ks],
                                                      rhs=Q[:, qs], start=True, stop=False),
                             reads=[Kp.b, Q.b], writes=[pS.b])
                        k.op("pe", lambda e: e.matmul(pS[:, u * 256:u * 256 + 128], lhsT=c["id_b"][:],
                                                      rhs=tbias[:, h, pi, 0, :], start=False, stop=True),
                             reads=[tbias.b, c["id_b"].b], writes=[pS.b])
                    k.op("pe", lambda e: e.matmul(pS[:, u * 256 + 128:u * 256 + 256], lhsT=Kp[:, qs],
                                                  rhs=Q[:, qs], start=True, stop=False),
                         reads=[Kp.b, Q.b], writes=[pS.b])
                    k.op("pe", lambda e: e.matmul(pS[:, u * 256 + 128:u * 256 + 256], lhsT=c["id_b"][:],
                                                  rhs=tbias[:, h, pi, 1, :], start=False, stop=True),
                         reads=[tbias.b, c["id_b"].b], writes=[pS.b])

            def stP(j):
                pi, d, nb, r, n0, per, n2, nu, bk, lastb = steps[j]
                pS, P, pO = pq[j % 2], pt[j % 2], po[bk % 2]
                lo = 128 if n2 == 0 else 0
                k.op("act", lambda e: e.activation(out=P[:, lo:256 * nu], in_=pS[:, lo:256 * nu],
                                                   func=AF.Exp), reads=[pS.b], writes=[P.b])
                for u in range(nu):
                    n = n2 + u
                    oc = (n - n0) * 128
                    if n > 0:
                        k.op("pe", lambda e: e.matmul(pO[0:65, oc:oc + 128],
                                                      lhsT=V[:, pi, r * nb + n - 1, ln, :],
                                                      rhs=P[:, u * 256:u * 256 + 128], start=True,
                                                      stop=False), reads=[V.b, P.b], writes=[pO.b])
                    k.op("pe", lambda e: e.matmul(pO[0:65, oc:oc + 128], lhsT=V[:, pi, r * nb + n, ln, :],
                                                  rhs=P[:, u * 256 + 128:u * 256 + 256], start=(n == 0),
                                                  stop=True), reads=[V.b, P.b], writes=[pO.b])
                if lastb:
                    s0 = 128 * n0 * d + r
                    dst = accs[:, s0:s0 + (128 * per - 1) * d + 1:d]
                    if pi == 0:
                        k.op("act", lambda e: e.activation(out=dst, in_=pO[0:65, 0:128 * per], func=AF.Identity),
                             reads=[pO.b], writes=[accs.b])
                    else:
                        k.op("dve", lambda e: e.tensor_tensor(out=dst, in0=dst, in1=pO[0:65, 0:128 * per],
                                                              op=ALU.add), reads=[pO.b], writes=[accs.b])

            stS(0)
            yield
            for j in range(len(steps)):
                if j + 1 < len(steps):
                    stS(j + 1)
                stP(j)
                yield
            pb = pq[0]
            for g in range(NG):
                c0 = g * 512
                O = R["ob"][g % 2]
                k.op("act", lambda e: e.activation(out=rl[64:65, :], in_=accs[64:65, c0:c0 + 512], func=AF.Ln),
                     reads=[accs.b], writes=[rl.b])
                k.op("act", lambda e: e.activation(out=rl[64:65, :], in_=rl[64:65, :], func=AF.Exp, scale=-1.0),
                     writes=[rl.b])
                k.op("pe", lambda e: e.matmul(pb[0:64, :], lhsT=c["ones_f"][64:65, 0:64], rhs=rl[64:65, :],
                                              start=True, stop=True), reads=[rl.b, c["ones_f"].b], writes=[pb.b])
                k.op("dve", lambda e: e.tensor_tensor(out=O[:], in0=accs[0:64, c0:c0 + 512], in1=pb[0:64, :],
                                                      op=ALU.mult), reads=[accs.b, pb.b], writes=[O.b])
                k.dma("sp", mixedT[MOFF + 64 * h:MOFF + 64 * h + 64, c0:c0 + 512], O[:], reads=[O.b],
                      writes=[mixedT.b])
                yield

        for pr_ in range(3):
            load_w(cx, wq, w_in, L, QOFF + 128 * pr_, 128)
            load_w(cx, wk, w_in, L, KOFF + 128 * pr_, 128)
            load_w(cx, wv, w_in, L, VOFF + 128 * pr_, 128)
            n = 0
            for which in range(2):
                W, gain = (wq, gq) if which == 0 else (wk, gk)
                for tg in range(NG):
                    cs = slice(tg * 512, (tg + 1) * 512)
                    p, pss = lanes[0]["pq"][tg % 2], lanes[1]["pq"][tg % 2]
                    s_, r_ = sq[n % 2], rr[n % 2]
                    n += 1
                    for kk in range(8):
                        k.op("pe", lambda e: e.matmul(p[:], lhsT=W[:, kk, :], rhs=hT[:, kk, cs],
                                                      start=(kk == 0), stop=(kk == 7)),
                             reads=[W.b, hT.b], writes=[p.b])
                    k.op("act", lambda e: e.activation(out=s_[:], in_=p[:], func=AF.Square), reads=[p.b], writes=[s_.b])
                    k.op("pe", lambda e: e.matmul(pss[:], lhsT=bd[:], rhs=s_[:], start=True, stop=True),
                         reads=[s_.b, bd.b], writes=[pss.b])
                    k.op("act", lambda e: e.activation(out=r_[:], in_=pss[:], func=AF.Ln, scale=1.0 / HD,
                                                       bias=c["eps"][:]), reads=[pss.b, c["eps"].b], writes=[r_.b])
                    k.op("act", lambda e: e.activation(out=r_[:], in_=r_[:], func=AF.Exp, scale=-0.5), writes=[r_.b])
                    if which == 0:
                        for half in range(2):
                            hs = slice(64 * half, 64 * half + 64)
                            k.op("dve", lambda e: e.scalar_tensor_tensor(out=Qp[half][hs, cs], in0=p[hs, :],
                                                                         scalar=gain[hs, :], in1=r_[hs, :],
                                                                         op0=ALU.mult, op1=ALU.mult),
                                 reads=[p.b, r_.b, gain.b], writes=[Qp[half].b])
                    else:
                        k.op("dve", lambda e: e.scalar_tensor_tensor(out=Kp[:, cs], in0=p[:], scalar=gain[:],
                                                                     in1=r_[:], op0=ALU.mult, op1=ALU.mult),
                             reads=[p.b, r_.b, gain.b], writes=[Kp.b])
            nv = 0
            for pi, (win, d) in enumerate(DIL_PATTERNS):
                nb = S // (128 * d)
                tiles = [(r, n) for r in range(d) for n in range(nb)]
                for t4 in range(0, len(tiles), 4):
                    p = lanes[nv % 2]["po"][(nv // 2) % 2]
                    nv += 1
                    for jj, (r, n) in enumerate(tiles[t4:t4 + 4]):
                        s0 = 128 * n * d + r
                        for kk in range(8):
                            k.op("pe", lambda e: e.matmul(p[:, jj * 128:(jj + 1) * 128],
                                                          lhsT=hT[:, kk, s0:s0 + 127 * d + 1:d], rhs=wv[:, kk, :],
                                                          start=(kk == 0), stop=(kk == 7)),
                                 reads=[hT.b, wv.b], writes=[p.b])
                    k.op("act", lambda e: e.activation(out=V[:, pi, t4:t4 + 4, :, 0:64],
                                                       in_=p[:].rearrange("p (j h e) -> p j h e", j=4, h=2),
                                                       func=AF.Identity), reads=[p.b], writes=[V.b])
            run_lanes([attn_gen(2 * pr_ + ln, ln, Qp[ln], lanes[ln]) for ln in range(2)])
        k.barrier()


def emit_wout(cx, c, L, modb, xsrc, xdst, w_out, mixedT):
    k = cx.k
    with contextlib.ExitStack() as es:
        wo = cx.sb("wo_w", [128, 8, D], BF16, es)
        mT = [cx.sb("wo_m%d" % i, [128, 8, 512], BF16, es) for i in range(2)]
        xin = [cx.sb("wo_x%d" % i, [128, D], F32, es) for i in range(3)]
        xo = [cx.sb("wo_o%d" % i, [128, D], F32, es) for i in range(3)]
        pw = [cx.ps("wo_p%d" % i, [128, 512], F32, es) for i in range(4)]
        k.dma("pool", wo[:], w_out[L].rearrange("(k p) d -> p k d", p=128), writes=[wo.b])
        def load_m(g):
            k.dma("sp", mT[g % 2][:], mixedT[:, g * 512:(g + 1) * 512].rearrange("(k p) t -> p k t", p=128),
                  reads=[mixedT.b], writes=[mT[g % 2].b])

        load_m(0)
        for g in range(cx.S // 512):
            m = mT[g % 2]
            if g + 1 < cx.S // 512:
                load_m(g + 1)
            for tt in range(4):
                i = g * 4 + tt
                x, o = xin[i % 3], xo[i % 3]
                k.dma("sp", x[:], xsrc(i), reads=[xsrc.b], writes=[x.b])
                for dh in range(2):
                    p = pw[(i % 2) * 2 + dh]
                    for kk in range(8):
                        k.op("pe", lambda e: e.matmul(p[:], lhsT=m[:, kk, tt * 128:(tt + 1) * 128],
                                                      rhs=wo[:, kk, dh * 512:(dh + 1) * 512],
                                                      start=(kk == 0), stop=(kk == 7)),
                             reads=[m.b, wo.b], writes=[p.b])
                    k.op("dve", lambda e: e.tensor_tensor(out=o[:, dh * 512:(dh + 1) * 512], in0=p[:],
                                                          in1=modb[:, 2048 + dh * 512:2048 + (dh + 1) * 512],
                                                          op=ALU.mult), reads=[p.b, modb.b], writes=[o.b])
                k.op("pool", lambda e: e.tensor_tensor(out=o[:], in0=o[:], in1=x[:], op=ALU.add),
                     reads=[x.b], writes=[o.b])
                k.dma("act", xdst(i), o[:], reads=[o.b], writes=[xdst.b])
        k.barrier()


def emit_mixer(cx, c, L, modb, xsrc, xdst, P, mixedT, parts=("fox", "dil", "sb")):
    if len(parts) < 3:
        xdst = None
    k = cx.k
    with contextlib.ExitStack() as es:
        hT = cx.sb("hT", [128, 8, cx.S], BF16, es)
        with contextlib.ExitStack() as es2:
            emit_norm(cx, c, es2, xsrc, modb, 1024, 0, hT, range(cx.NT), 0)
            k.barrier()
        if "fox" in parts:
            emit_fox(cx, c, L, hT, P["w_in"], P["b_forget"], P["q_gain_fox"], P["k_gain_fox"], mixedT)
        if "dil" in parts:
            emit_dil(cx, c, L, hT, P["w_in"], P["q_gain_dil"], P["k_gain_dil"], P["tb_gather"], P["tb_mask"], mixedT)
        if "sb" in parts:
            emit_sb(cx, c, L, hT, P["w_in"], mixedT)
        k.barrier()
    if xdst is not None:
        emit_wout(cx, c, L, modb, xsrc, xdst, P["w_out"], mixedT)


def emit_ffn_sparse(cx, c, L, modb, xsrc, xdst, router_w, router_b, w_gate, w_up, w_down, scr):
    k = cx.k
    NT = cx.NT
    NBT = cx.S // 512
    NB = NBT + 3
    I32 = mybir.dt.int32
    h2s, combs, ys = scr["h2s"], scr["combs"], scr["ys"]
    wg_rows = w_gate.rearrange("l e (p h k) f -> (l e p h) (k f)", h=4, k=2)
    wu_rows = w_up.rearrange("l e (p h k) f -> (l e p h) (k f)", h=4, k=2)
    wd_rows = w_down.rearrange("l e (p h c) d -> (l e p h) (c d)", h=4, c=1)
    with contextlib.ExitStack() as es:
        pos_i = cx.sb("s_posi", [128, NT], I32, es)
        idxE = cx.sb("s_idxE", [128, NB, 16], I32, es)
        zer = cx.sb("s_zero", [128, 256], F32, es)
        k.op("pool", lambda e: e.memset(zer[:], 0.0), writes=[zer.b])
        k.dma("pool", combs[:, :].rearrange("(p r) c -> p (r c)", p=128), zer[:, 0:NB * 16], reads=[zer.b],
              writes=[combs.b])
        with contextlib.ExitStack() as es1:
            h2tok = cx.sb("s_h2tok", [128, NT, D], BF16, es1)
            rw = cx.sb("f_rw", [128, 8, 16], F32, es1)
            rb = cx.sb("f_rb", [128, 16], F32, es1)
            rb1 = cx.sb("f_rb1", [1, 16], F32, es1)
            logits = cx.sb("f_logits", [128, NT, 16], F32, es1)
            comb = cx.sb("f_comb", [128, NT, 16], F32, es1)
            G = cx.sb("s_G", [128, NT, 4], F32, es1)
            combg = cx.sb("s_combg", [128, NT, 4], F32, es1)
            k.dma("sp", rw[:], router_w.rearrange("(k p) e -> p k e", p=128), writes=[rw.b])
            k.dma("sp", rb1[:], router_b, writes=[rb1.b])
            with contextlib.ExitStack() as es2:
                prb = cx.ps("f_prb", [128, 16], F32, es2)
                k.op("pe", lambda e: e.matmul(prb[:], lhsT=c["ones_f"][0:1, :], rhs=rb1[0:1, :], start=True, stop=True),
                     reads=[c["ones_f"].b, rb1.b], writes=[prb.b])
                k.op("act", lambda e: e.activation(out=rb[:], in_=prb[:], func=AF.Identity), reads=[prb.b],
                     writes=[rb.b])
                k.barrier()
            with contextlib.ExitStack() as es2:
                emit_norm(cx, c, es2, xsrc, modb, 4096, 3072, None, range(NT), 0,
                          router=dict(rw=rw, rb=rb, logits=logits), rows_dst=h2tok)
                k.barrier()
            with contextlib.ExitStack() as es2:
                emit_router(cx, es2, logits, comb, 0, NT, G=G, combg=combg)
                Gb = cx.sb("s_Gb", [128, NT * 4], BF16, es2)
                tri = cx.sb("s_tri", [128, 128], BF16, es2)
                cnt = cx.sb("s_cnt", [128, NT, 4], F32, es2)
                inc = cx.sb("s_inc", [128, NT, 4], F32, es2)
                tot = cx.sb("s_tot", [128, NT, 4], F32, es2)
                thr = cx.sb("s_thr", [128, NB], F32, es2)
                thr_i = cx.sb("s_thri", [128, NB], I32, es2)
                cmp_ = cx.sb("s_cmp", [128, 4, NBT], F32, es2)
                nbg = cx.sb("s_nbg", [128, 4], F32, es2)
                po = cx.sb("s_po", [128, 4], F32, es2)
                cmpb = cx.sb("s_cmpb", [128, NB, 3], F32, es2)
                gb = cx.sb("s_gb", [128, NB], F32, es2)
                posf = cx.sb("s_posf", [128, NT], F32, es2)
                cE_i = cx.sb("s_cEi", [128, 16], I32, es2)
                cE = cx.sb("s_cE", [128, 16], F32, es2)
                fE = cx.sb("s_fE", [128, NB, 16], F32, es2)
                prk = cx.ps("s_prk", [128, NT * 4], F32, es2)
                pcn = cx.ps("s_pcn", [128, NT * 4], F32, es2)
                k.op("pool", lambda e: e.affine_select(out=tri[:], in_=c["ones_b"][:], pattern=[[1, 128]],
                                                       compare_op=ALU.is_gt, fill=0.0, base=0, channel_multiplier=-1),
                     reads=[c["ones_b"].b], writes=[tri.b])
                k.op("pool", lambda e: e.iota(thr_i[:], pattern=[[512, NB]], base=0, channel_multiplier=0),
                     writes=[thr_i.b])
                k.op("dve", lambda e: e.tensor_copy(out=thr[:], in_=thr_i[:]), reads=[thr_i.b], writes=[thr.b])
                k.op("pool", lambda e: e.iota(cE_i[:], pattern=[[512, 4], [1, 4]], base=L * NEXP * 512, channel_multiplier=4),
                     writes=[cE_i.b])
                k.op("dve", lambda e: e.tensor_copy(out=cE[:], in_=cE_i[:]), reads=[cE_i.b], writes=[cE.b])
                k.op("dve", lambda e: e.tensor_copy(out=Gb[:], in_=G[:].rearrange("p n g -> p (n g)")),
                     reads=[G.b], writes=[Gb.b])
                k.op("pe", lambda e: e.matmul(prk[:], lhsT=tri[:], rhs=Gb[:], start=True, stop=True),
                     reads=[tri.b, Gb.b], writes=[prk.b])
                k.op("pe", lambda e: e.matmul(pcn[:], lhsT=c["ones_b"][:], rhs=Gb[:], start=True, stop=True),
                     reads=[c["ones_b"].b, Gb.b], writes=[pcn.b])
                k.op("act", lambda e: e.activation(out=cnt[:].rearrange("p n g -> p (n g)"), in_=pcn[:],
                                                   func=AF.Identity), reads=[pcn.b], writes=[cnt.b])
                for g in range(4):
                    k.op("dve", lambda e: e.tensor_tensor_scan(out=inc[:, :, g], data0=cnt[:, :, g], data1=cnt[:, :, g],
                                                               initial=0.0, op0=ALU.add, op1=ALU.max),
                         reads=[cnt.b], writes=[inc.b])
                k.op("dve", lambda e: e.tensor_tensor(out=cmp_[:],
                                                      in0=inc[:, NT - 1, :].unsqueeze(2).to_broadcast([128, 4, NBT]),
                                                      in1=thr[:, 0:NBT].unsqueeze(1).to_broadcast([128, 4, NBT]),
                                                      op=ALU.is_gt), reads=[inc.b, thr.b], writes=[cmp_.b])
                k.op("dve", lambda e: e.tensor_reduce(out=nbg[:], in_=cmp_[:], axis=AX.X, op=ALU.add),
                     reads=[cmp_.b], writes=[nbg.b])
                k.op("pool", lambda e: e.memset(po[:], 0.0), writes=[po.b])
                for g in range(1, 4):
                    k.op("dve", lambda e: e.scalar_tensor_tensor(out=po[:, g:g + 1], in0=nbg[:, g - 1:g], scalar=512.0,
                                                                 in1=po[:, g - 1:g], op0=ALU.mult, op1=ALU.add),
                         reads=[nbg.b], writes=[po.b])
                k.op("dve", lambda e: e.tensor_tensor(out=tot[:], in0=inc[:], in1=cnt[:], op=ALU.subtract),
                     reads=[inc.b, cnt.b], writes=[tot.b])
                k.op("dve", lambda e: e.tensor_tensor(out=tot[:], in0=tot[:],
                                                      in1=po[:].unsqueeze(1).to_broadcast([128, NT, 4]), op=ALU.add),
                     reads=[po.b], writes=[tot.b])
                k.op("dve", lambda e: e.tensor_tensor(out=tot[:].rearrange("p n g -> p (n g)"),
                                                      in0=tot[:].rearrange("p n g -> p (n g)"), in1=prk[:], op=ALU.add),
                     reads=[prk.b], writes=[tot.b])
                k.op("dve", lambda e: e.tensor_tensor(out=tot[:], in0=tot[:], in1=G[:], op=ALU.mult),
                     reads=[G.b], writes=[tot.b])
                k.op("dve", lambda e: e.tensor_reduce(out=posf[:], in_=tot[:], axis=AX.X, op=ALU.add),
                     reads=[tot.b], writes=[posf.b])
                k.op("dve", lambda e: e.tensor_copy(out=pos_i[:], in_=posf[:]), reads=[posf.b], writes=[pos_i.b])
                k.op("dve", lambda e: e.tensor_tensor(out=cmpb[:],
                                                      in0=po[:, 1:4].unsqueeze(1).to_broadcast([128, NB, 3]),
                                                      in1=thr[:].unsqueeze(2).to_broadcast([128, NB, 3]),
                                                      op=ALU.is_le), reads=[po.b, thr.b], writes=[cmpb.b])
                k.op("dve", lambda e: e.tensor_reduce(out=gb[:], in_=cmpb[:], axis=AX.X, op=ALU.add),
                     reads=[cmpb.b], writes=[gb.b])
                k.op("dve", lambda e: e.scalar_tensor_tensor(out=fE[:], in0=gb[:].unsqueeze(2).to_broadcast([128, NB, 16]),
                                                             scalar=2048.0,
                                                             in1=cE[:].unsqueeze(1).to_broadcast([128, NB, 16]),
                                                             op0=ALU.mult, op1=ALU.add),
                     reads=[gb.b, cE.b], writes=[fE.b])
                k.op("dve", lambda e: e.tensor_copy(out=idxE[:], in_=fE[:]), reads=[fE.b], writes=[idxE.b])
                for i in range(NT):
                    k.dma("pool", None, None, reads=[h2tok.b, pos_i.b], writes=[h2s.b], nowaw=True,
                          fn=lambda eng: eng.indirect_dma_start(
                              out=h2s[:, :], out_offset=bass.IndirectOffsetOnAxis(ap=pos_i[:, i:i + 1], axis=0),
                              in_=h2tok[:, i, :], in_offset=None))
                    k.dma("pool", None, None, reads=[combg.b, pos_i.b], writes=[combs.b], nowaw=True,
                          fn=lambda eng: eng.indirect_dma_start(
                              out=combs[:, :], out_offset=bass.IndirectOffsetOnAxis(ap=pos_i[:, i:i + 1], axis=0),
                              in_=combg[:, i, :], in_offset=None))
                k.barrier()
        with contextlib.ExitStack() as es1:
            rows = [cx.sb("s_rows", [128, 4, D], BF16, es1) for i in range(2)]
            hTb = [cx.sb("s_hT", [128, 8, 512], BF16, es1) for i in range(2)]
            cmb = [cx.sb("s_cmb", [128, 4, 4], F32, es1) for i in range(2)]
            yb = [cx.sb("s_y", [128, 4, D], F32, es1) for i in range(2)]
            NW = 3
            wg = [cx.sb("m_wg", [128, 8, DFF], BF16, es1) for i in range(NW)]
            wu = [cx.sb("m_wu", [128, 8, DFF], BF16, es1) for i in range(NW)]
            wd = [cx.sb("m_wd", [128, 4, D], BF16, es1) for i in range(NW)]
            sg = [cx.sb("m_sg", [128, 512], F32, es1) for i in range(2)]
            hid = [cx.sb("m_hid", [128, 4, 512], BF16, es1) for i in range(2)]
            pg = [cx.ps("m_pg", [128, 512], F32, es1) for i in range(2)]
            pu = [cx.ps("m_pu", [128, 512], F32, es1) for i in range(2)]
            pd = [cx.ps("m_pd", [128, 512], F32, es1) for i in range(2)]
            ptr = [cx.ps("s_ptr", [128, 8, 128], BF16, es1) for i in range(2)]

            def load_expert(b, j, n):
                i = n % NW
                for hh in range(4):
                    off = bass.IndirectOffsetOnAxis(ap=idxE[:, b, 4 * j + hh:4 * j + hh + 1], axis=0)
                    k.dma("pool", None, None, reads=[idxE.b], writes=[wg[i].b], nowaw=True,
                          fn=lambda eng: eng.indirect_dma_start(
                              out=wg[i][:, 2 * hh:2 * hh + 2, :].rearrange("p k f -> p (k f)"), out_offset=None,
                              in_=wg_rows, in_offset=off))
                    k.dma("pool", None, None, reads=[idxE.b], writes=[wu[i].b], nowaw=True,
                          fn=lambda eng: eng.indirect_dma_start(
                              out=wu[i][:, 2 * hh:2 * hh + 2, :].rearrange("p k f -> p (k f)"), out_offset=None,
                              in_=wu_rows, in_offset=off))
                    k.dma("pool", None, None, reads=[idxE.b], writes=[wd[i].b], nowaw=True,
                          fn=lambda eng: eng.indirect_dma_start(
                              out=wd[i][:, hh, :], out_offset=None,
                              in_=wd_rows, in_offset=off))

            def prep_block(b):
                R_, H_, C_ = rows[b % 2], hTb[b % 2], cmb[b % 2]
                k.dma("sp", R_[:], h2s[b * 512:(b + 1) * 512, :].rearrange("(j p) d -> p j d", p=128),
                      reads=[h2s.b], writes=[R_.b])
                k.dma("sp", C_[:], combs[b * 512:(b + 1) * 512, :].rearrange("(j p) c -> p j c", p=128),
                      reads=[combs.b], writes=[C_.b])
                for tt in range(4):
                    p = ptr[tt % 2]
                    for kk in range(8):
                        k.op("pe", lambda e: e.transpose(p[:, kk, :], R_[:, tt, kk:kk + 1017:8],
                                                         c["id_b"][:]), reads=[R_.b, c["id_b"].b], writes=[p.b])
                    k.op("dve", lambda e: e.tensor_copy(out=H_[:, :, tt * 128:(tt + 1) * 128], in_=p[:]),
                         reads=[p.b], writes=[H_.b])

            work = [(b, j) for b in range(NB) for j in range(4)]
            load_expert(0, 0, 0)
            load_expert(work[1][0], work[1][1], 1)
            for n, (b, j) in enumerate(work):
                if n + 2 < len(work):
                    load_expert(work[n + 2][0], work[n + 2][1], n + 2)
                R_, H_, C_, Y_ = rows[b % 2], hTb[b % 2], cmb[b % 2], yb[b % 2]
                if n == 0:
                    prep_block(0)
                if j == 2 and b + 1 < NB:
                    prep_block(b + 1)
                g_, u_, d_ = wg[n % NW], wu[n % NW], wd[n % NW]
                hd = hid[n % 2]
                for fc in range(4):
                    pG, pU, s_ = pg[fc % 2], pu[fc % 2], sg[fc % 2]
                    for kk in range(8):
                        k.op("pe", lambda en: en.matmul(pG[:], lhsT=g_[:, kk, fc:fc + 509:4],
                                                        rhs=H_[:, kk, :], start=(kk == 0), stop=(kk == 7)),
                             reads=[g_.b, H_.b], writes=[pG.b])
                    for kk in range(8):
                        k.op("pe", lambda en: en.matmul(pU[:], lhsT=u_[:, kk, fc:fc + 509:4],
                                                        rhs=H_[:, kk, :], start=(kk == 0), stop=(kk == 7)),
                             reads=[u_.b, H_.b], writes=[pU.b])
                    k.op("act", lambda en: en.activation(out=s_[:], in_=pG[:], func=AF.Silu),
                         reads=[pG.b], writes=[s_.b])
                    k.op("dve", lambda en: en.tensor_tensor(out=hd[:, fc, :], in0=pU[:], in1=s_[:], op=ALU.mult),
                         reads=[pU.b, s_.b], writes=[hd.b])
                for tt in range(4):
                    for dh in range(2):
                        pD = pd[dh]
                        for fc in range(4):
                            k.op("pe", lambda en: en.matmul(pD[:], lhsT=hd[:, fc, tt * 128:(tt + 1) * 128],
                                                            rhs=d_[:, fc, dh * 512:(dh + 1) * 512],
                                                            start=(fc == 0), stop=(fc == 3)),
                                 reads=[hd.b, d_.b], writes=[pD.b])
                        a = Y_[:, tt, dh * 512:(dh + 1) * 512]
                        if j == 0:
                            k.op("dve", lambda en: en.tensor_scalar(out=a, in0=pD[:], scalar1=C_[:, tt, j:j + 1],
                                                                    scalar2=None, op0=ALU.mult),
                                 reads=[pD.b, C_.b], writes=[Y_.b])
                        else:
                            k.op("dve", lambda en: en.scalar_tensor_tensor(out=a, in0=pD[:], scalar=C_[:, tt, j:j + 1],
                                                                           in1=a, op0=ALU.mult, op1=ALU.add),
                                 reads=[pD.b, C_.b], writes=[Y_.b])
                if j == 3:
                    k.dma("sp", ys[b * 512:(b + 1) * 512, :].rearrange("(j p) d -> p j d", p=128), Y_[:],
                          reads=[Y_.b], writes=[ys.b])
            k.barrier()
        with contextlib.ExitStack() as es1:
            xin = [cx.sb("o_xin", [128, D], F32, es1) for i in range(4)]
            yg = [cx.sb("o_yg", [128, D], F32, es1) for i in range(4)]
            def fetch(i):
                x, y_ = xin[i % 4], yg[i % 4]
                k.dma("sp", x[:], xsrc(i), reads=[xsrc.b], writes=[x.b])
                k.dma("pool", None, None, reads=[ys.b, pos_i.b], writes=[y_.b],
                      fn=lambda eng: eng.indirect_dma_start(
                          out=y_[:], out_offset=None, in_=ys[:, :],
                          in_offset=bass.IndirectOffsetOnAxis(ap=pos_i[:, i:i + 1], axis=0)))

            for i in range(min(3, NT)):
                fetch(i)
            for i in range(NT):
                x, y_ = xin[i % 4], yg[i % 4]
                k.op("dve", lambda e: e.tensor_tensor(out=y_[:], in0=y_[:], in1=modb[:, 5120:6144], op=ALU.mult),
                     reads=[modb.b], writes=[y_.b])
                k.op("dve", lambda e: e.tensor_tensor(out=y_[:], in0=y_[:], in1=x[:], op=ALU.add),
                     reads=[x.b], writes=[y_.b])
                k.dma("act", xdst(i), y_[:], reads=[y_.b], writes=[xdst.b])
                if i + 3 < NT:
                    fetch(i + 3)
            k.barrier()


class TileSrc:
    def __init__(self, t):
        self.t = t
        self.b = t.b

    def __call__(self, i):
        return self.t[i * 128:(i + 1) * 128, :]


def build(S=4096, layers=(0, 1), dbg=None, phases=("mix", "ffn")):
    nc = bass.Bass("TRN2", target_bir_lowering=False)
    es = contextlib.ExitStack()
    with es:
        cx = Ctx(nc, es, S, dbg or {})
        k = cx.k

        def inp(name, shape):
            return nc.dram_tensor(name, list(shape), F32, kind="ExternalInput").ap()

        x = T(inp("x", [S, D]), k.buf("x"))
        cT = inp("cT", [128, 8])
        ada_w = inp("ada_w", [DEPTH, D, 6 * D])
        ada_b = inp("ada_b", [DEPTH, 6 * D])
        norm_mix = inp("norm_mix", [DEPTH, D])
        norm_ffn = inp("norm_ffn", [DEPTH, D])
        router_w = inp("router_w", [D, NEXP])
        router_b = inp("router_b", [1, NEXP])
        w_gate = inp("w_gate", [DEPTH, NEXP, D, DFF])
        w_up = inp("w_up", [DEPTH, NEXP, D, DFF])
        w_down = inp("w_down", [DEPTH, NEXP, DFF, D])
        P = dict(w_in=inp("w_in", [DEPTH, D, IN_COLS]), b_forget=inp("b_forget", [DEPTH, 4]),
                 q_gain_fox=inp("q_gain_fox", [DEPTH, HD]), k_gain_fox=inp("k_gain_fox", [DEPTH, HD]),
                 q_gain_dil=inp("q_gain_dil", [DEPTH, HD]), k_gain_dil=inp("k_gain_dil", [DEPTH, HD]),
                 w_out=inp("w_out", [DEPTH, D, D]))
        tb_gather = inp("tb_gather", [128, 6, 768])
        tb_mask = inp("tb_mask", [128, 768])
        out = T(nc.dram_tensor("out", [S, D], F32, kind="ExternalOutput").ap(), k.buf("out"))
        xres = cx.dram("xres", [S, D], F32)
        xmid = cx.dram("xmid", [S, D], F32)
        if "mixedT" in cx.dbg:
            mixedT = T(nc.dram_tensor("mixedT", [D, S], BF16, kind="ExternalOutput").ap(), k.buf("mixedT"))
        else:
            mixedT = cx.dram("mixedT", [D, S], BF16)

        c = emit_consts(cx)
        modb = cx.sb("modb", [128, 6 * D], F32)
        sparse = "dense" not in cx.dbg
        if sparse:
            NB = S // 512 + 3
            scr = dict(h2s=cx.dram("h2s", [NB * 512, D], BF16), combs=cx.dram("combs", [NB * 512, 4], F32),
                       ys=cx.dram("ys", [NB * 512, D], F32))
            with contextlib.ExitStack() as es2:
                zb = cx.sb("zb", [128, 4 * D], BF16, es2)
                k.op("pool", lambda e: e.memset(zb[:], 0.0), writes=[zb.b])
                for b in range(NB):
                    k.dma("pool", scr["h2s"][b * 512:(b + 1) * 512, :].rearrange("(p r) d -> p (r d)", p=128), zb[:],
                          reads=[zb.b], writes=[scr["h2s"].b])
                k.barrier()
        P["tb_gather"], P["tb_mask"] = tb_gather, tb_mask
        cur = x
        for li, L in enumerate(layers):
            last = li == len(layers) - 1
            emit_mod(cx, c, L, modb, cT, ada_w, ada_b, norm_mix, norm_ffn)
            dst = out if last else xres
            if "mix" in phases:
                mdst = xmid if "ffn" in phases else dst
                emit_mixer(cx, c, L, modb, TileSrc(cur), TileSrc(mdst), P, mixedT, parts=cx.dbg.get("parts", ("fox", "dil", "sb")))
                cur = mdst
            if "ffn" in phases and sparse:
                emit_ffn_sparse(cx, c, L, modb, TileSrc(cur), TileSrc(dst), router_w, router_b, w_gate, w_up, w_down,
                                scr)
            elif "ffn" in phases:
                emit_ffn(cx, c, L, modb, TileSrc(cur), TileSrc(dst), router_w, router_b, w_gate, w_up, w_down)
            cur = dst
        k.finish([out.b])
    return nc


def dil_bias_tables(rel_bias):
    kk = np.arange(128)[:, None]
    qq = np.arange(128)[None, :]
    idx = np.zeros((3, 2, 128, 128), np.int64)
    msk = np.zeros((3, 2, 128, 128), np.float32)
    for pi, (win, d) in enumerate(DIL_PATTERNS):
        span = win // d
        for cc in range(2):
            steps = qq + 128 - (kk + 128 * cc)
            band = (steps >= 0) & (steps <= span)
            dist = np.maximum(steps, 0) * d
            dd = np.maximum(dist, 16).astype(np.float64)
            large = 16 + (np.log(dd / 16.0) / np.log(2048.0 / 16.0) * 16.0).astype(np.int64)
            large = np.minimum(large, 31)
            bucket = np.where(dist < 16, dist, large)
            idx[pi, cc] = np.where(band, bucket, 0)
            msk[pi, cc] = np.where(band, 0.0, NEG)
    g = rel_bias[idx]
    g = np.ascontiguousarray(np.transpose(g, (2, 4, 0, 1, 3))).reshape(128, 6, 768)
    m = np.ascontiguousarray(np.transpose(msk, (2, 0, 1, 3))).reshape(128, 768)
    return g.astype(np.float32), m


def make_in_maps(inputs, S=4096):
    f = lambda a: np.ascontiguousarray(np.asarray(a, dtype=np.float32))
    shared = {n: f(inputs[n]) for n in ("ada_w", "ada_b", "norm_mix", "norm_ffn", "router_w",
                                        "w_gate", "w_up", "w_down", "w_in", "b_forget", "q_gain_fox",
                                        "k_gain_fox", "q_gain_dil", "k_gain_dil", "w_out")}
    shared["tb_gather"], shared["tb_mask"] = dil_bias_tables(f(inputs["rel_bias"]))
    shared["router_b"] = f(inputs["router_b"]).reshape(1, NEXP)
    maps = []
    for b in range(NCORES):
        m = dict(shared)
        m["x"] = f(inputs["x"][b])
        m["cT"] = np.ascontiguousarray(f(inputs["c"][b]).reshape(8, 128).T)
        maps.append(m)
    return maps


def kernel(**inputs):
    nc = build()
    in_maps = make_in_maps(inputs)
    res = run_bass_kernel_spmd(nc, in_maps, core_ids=list(range(NCORES)))
    return np.stack([np.asarray(r["out"]) for r in res.results], axis=0).astype(np.float32)
```

```python
import bisect
import contextlib
import numpy as np
import concourse.bass as bass
import concourse.mybir as mybir
from concourse.bass_utils import run_bass_kernel_spmd

F32 = mybir.dt.float32
BF16 = mybir.dt.bfloat16
AF = mybir.ActivationFunctionType
ALU = mybir.AluOpType
AX = mybir.AxisListType

D = 1024
DEPTH = 2
HD = 64
NCORES = 8
IN_COLS = 3332
NEXP = 16
DFF = 512
EPS = 1e-6
NEG = -30000.0


class Buf:
    __slots__ = ("name", "w", "r", "sem", "semv", "semk")

    def __init__(self, name):
        self.name = name
        self.w = None
        self.r = {}
        self.sem = None
        self.semv = 0
        self.semk = None


class Eng:
    def __init__(self, name, eng, sem):
        self.name = name
        self.eng = eng
        self.sem = sem
        self.seq = 0
        self.nsig = 0
        self.sig_seq = []
        self.sig_idx = []
        self.last = None
        self.seen = {}


class PEProxy:
    def __init__(self, eng):
        self.eng = eng
        self.stop = False

    def matmul(self, *a, **kw):
        self.stop = bool(kw.get("stop"))
        return self.eng.matmul(*a, **kw)

    def transpose(self, *a, **kw):
        self.stop = False
        return self.eng.transpose(*a, **kw)


class Sched:
    def __init__(self, nc, es):
        self.nc = nc
        self.es = es
        self.E = {}
        for name, eng in (("pe", nc.tensor), ("act", nc.scalar), ("dve", nc.vector),
                          ("pool", nc.gpsimd), ("sp", nc.sync)):
            sem = es.enter_context(nc.semaphore("sem_" + name))
            self.E[name] = Eng(name, eng, sem)
        self.pep = PEProxy(nc.tensor)
        self.bufs = {}
        self.nsem = 0
        self.sem_pool = {}

    def buf(self, name):
        return Buf(name)

    def _resolve(self, en, seq):
        E = self.E[en]
        i = bisect.bisect_left(E.sig_seq, seq)
        if i < len(E.sig_seq):
            return E.sem, E.sig_idx[i]
        assert E.seq >= seq and E.last is not None
        E.nsig += 1
        E.last.then_inc(E.sem, 1)
        E.sig_seq.append(E.seq)
        E.sig_idx.append(E.nsig)
        return E.sem, E.nsig

    def _wait(self, en, toks):
        C = self.E[en]
        emax = {}
        dmax = {}
        for t in toks:
            if t is None:
                continue
            if t[0] == "E":
                if t[1] == "pe" and en == "pe":
                    continue
                if emax.get(t[1], 0) < t[2]:
                    emax[t[1]] = t[2]
            else:
                k = t[1].name
                if k not in dmax or dmax[k][1] < t[2]:
                    dmax[k] = (t[1], t[2])
        waits = []
        for pn, seq in emax.items():
            sem, val = self._resolve(pn, seq)
            waits.append((sem.name, sem, val))
        for k, (b, val) in dmax.items():
            waits.append((b.sem.name, b.sem, val))
        for key, sem, val in waits:
            if C.seen.get(key, 0) < val:
                C.eng.wait_ge(sem, val)
                C.seen[key] = val

    @staticmethod
    def _collect(reads, writes):
        toks = []
        for b in reads:
            toks.append(b.w)
        for b in writes:
            toks.append(b.w)
            toks.extend(b.r.values())
        return toks

    def _mark(self, tok, key, reads, writes):
        for b in reads:
            b.r[key] = tok
            self.bufs[id(b)] = b
        for b in writes:
            b.w = tok
            b.r = {}
            self.bufs[id(b)] = b

    def op(self, en, fn, reads=(), writes=(), sig=None):
        self._wait(en, self._collect(reads, writes))
        E = self.E[en]
        if en == "pe":
            self.pep.stop = False
            ins = fn(self.pep)
            if sig is None:
                sig = self.pep.stop
        else:
            ins = fn(E.eng)
        E.seq += 1
        E.last = ins
        if sig or (sig is None and en != "pe"):
            E.nsig += 1
            ins.then_inc(E.sem, 1)
            E.sig_seq.append(E.seq)
            E.sig_idx.append(E.nsig)
        self._mark(("E", en, E.seq), "E" + en, reads, writes)
        return ins

    def dma(self, qn, out, in_, reads=(), writes=(), fn=None, nowaw=False, **kw):
        toks = self._collect(reads, writes)
        if nowaw:
            b0 = writes[0]
            toks = [t for t in toks if not (t is not None and t[0] == "D" and t[1] is b0 and t is b0.w)]
        self._wait(qn, toks)
        E = self.E[qn]
        b0 = writes[0]
        kind = "sw" if qn == "pool" else "hw"
        if b0.sem is not None and b0.semk != kind:
            raise AssertionError("buffer %s written by both SW and HW DMA queues" % b0.name)
        if b0.sem is None:
            b0.semk = kind
            pool = self.sem_pool.setdefault(kind, [])
            if pool:
                b0.sem, b0.semv = pool.pop()
            else:
                self.nsem += 1
                b0.sem = self.es.enter_context(self.nc.semaphore("dsem%d" % self.nsem))
                b0.semv = 0
        ins = fn(E.eng) if fn is not None else E.eng.dma_start(out=out, in_=in_, **kw)
        b0.semv += 16
        ins.then_inc(b0.sem, 16)
        self._mark(("D", b0, b0.semv), "D" + b0.sem.name, reads, writes)
        return ins

    def barrier(self):
        toks = []
        for b in self.bufs.values():
            toks.append(b.w)
            toks.extend(b.r.values())
        for en, E in self.E.items():
            if E.seq > 0:
                toks.append(("E", en, E.seq))
        for en in self.E:
            self._wait(en, toks)
        for b in self.bufs.values():
            b.w = None
            b.r = {}
            if b.sem is not None:
                self.sem_pool.setdefault(b.semk, []).append((b.sem, b.semv))
                b.sem = None
        self.bufs = {}

    def finish(self, out_bufs):
        toks = []
        for b in out_bufs:
            toks.append(b.w)
        self._wait("sp", toks)


class T:
    def __init__(self, h, b):
        self.h = h
        self.b = b

    def __getitem__(self, idx):
        return self.h[idx]


class Ctx:
    def __init__(self, nc, es, S, dbg):
        self.nc = nc
        self.es = es
        self.S = S
        self.NT = S // 128
        self.k = Sched(nc, es)
        self.dbg = dbg
        self.dbg_out = {}

    def uname(self, name):
        self.uid = getattr(self, "uid", 0) + 1
        return "%s_%d" % (name, self.uid)

    def sb(self, name, shape, dt, es=None):
        name = self.uname(name)
        h = (es or self.es).enter_context(self.nc.sbuf_tensor(name, list(shape), dt))
        return T(h, self.k.buf(name))

    def ps(self, name, shape, dt=F32, es=None):
        name = self.uname(name)
        h = (es or self.es).enter_context(self.nc.psum_tensor(name, list(shape), dt))
        return T(h, self.k.buf(name))

    def dram(self, name, shape, dt, kind="Internal"):
        h = self.nc.dram_tensor(name, list(shape), dt, kind=kind)
        return T(h.ap(), self.k.buf(name))


def emit_consts(cx):
    k = cx.k
    c = {}
    c["ones_f"] = cx.sb("ones_f", [128, 128], F32)
    k.op("pool", lambda e: e.memset(c["ones_f"][:], 1.0), writes=[c["ones_f"].b])
    c["ones_b"] = cx.sb("ones_b", [128, 128], BF16)
    k.op("pool", lambda e: e.memset(c["ones_b"][:], 1.0), writes=[c["ones_b"].b])
    c["id_f"] = cx.sb("id_f", [128, 128], F32)
    k.op("pool", lambda e: e.affine_select(out=c["id_f"][:], in_=c["ones_f"][:], pattern=[[1, 128]],
                                           compare_op=ALU.is_equal, fill=0.0, base=0, channel_multiplier=-1),
         reads=[c["ones_f"].b], writes=[c["id_f"].b])
    c["id_b"] = cx.sb("id_b", [128, 128], BF16)
    k.op("pool", lambda e: e.tensor_copy(out=c["id_b"][:], in_=c["id_f"][:]),
         reads=[c["id_f"].b], writes=[c["id_b"].b])
    c["tri_ge"] = cx.sb("tri_ge", [128, 128], BF16)
    k.op("pool", lambda e: e.affine_select(out=c["tri_ge"][:], in_=c["ones_b"][:], pattern=[[-1, 128]],
                                           compare_op=ALU.is_ge, fill=0.0, base=0, channel_multiplier=1),
         reads=[c["ones_b"].b], writes=[c["tri_ge"].b])
    c["zeros_b"] = cx.sb("zeros_b", [128, 512], BF16)
    k.op("pool", lambda e: e.memset(c["zeros_b"][:], 0.0), writes=[c["zeros_b"].b])
    c["eps"] = cx.sb("c_eps", [128, 1], F32)
    k.op("pool", lambda e: e.memset(c["eps"][:], EPS), writes=[c["eps"].b])
    c["one"] = cx.sb("c_one", [128, 1], F32)
    k.op("pool", lambda e: e.memset(c["one"][:], 1.0), writes=[c["one"].b])
    return c


def emit_mod(cx, c, L, modb, cT, ada_w, ada_b, norm_mix, norm_ffn):
    k, nc = cx.k, cx.nc
    with contextlib.ExitStack() as es:
        cond = cx.sb("cond", [128, 8], F32, es)
        sig = cx.sb("csig", [128, 8], F32, es)
        condB = cx.sb("condB", [128, 8, 128], F32, es)
        wt = [cx.sb("adaw%d" % i, [128, 8, 512], F32, es) for i in range(2)]
        brow = cx.sb("adab", [1, 6144], F32, es)
        nrow = cx.sb("nrow", [1, 2048], F32, es)
        pm = [cx.ps("pmod%d" % i, [128, 512], F32, es) for i in range(2)]
        k.dma("sp", cond[:], cT, writes=[cond.b])
        k.dma("sp", brow[:], ada_b[L:L + 1, :], writes=[brow.b])
        k.dma("sp", nrow[:, 0:1024], norm_mix[L:L + 1, :], writes=[nrow.b])
        k.dma("sp", nrow[:, 1024:2048], norm_ffn[L:L + 1, :], writes=[nrow.b])
        k.op("act", lambda e: e.activation(out=sig[:], in_=cond[:], func=AF.Sigmoid),
             reads=[cond.b], writes=[sig.b])
        k.op("dve", lambda e: e.tensor_tensor(out=cond[:], in0=cond[:], in1=sig[:], op=ALU.mult),
             reads=[sig.b], writes=[cond.b])
        for kk in range(8):
            k.op("dve", lambda e, kk=kk: e.tensor_scalar(out=condB[:, kk, :], in0=c["ones_f"][:],
                                                        scalar1=cond[:, kk:kk + 1], scalar2=None, op0=ALU.mult),
                 reads=[cond.b, c["ones_f"].b], writes=[condB.b])
        for j in range(12):
            w = wt[j % 2]
            p = pm[j % 2]
            src = ada_w[L, :, j * 512:(j + 1) * 512].rearrange("(k p) f -> p k f", p=128)
            k.dma("sp", w[:], src, writes=[w.b])
            for kk in range(8):
                k.op("pe", lambda e, kk=kk: e.matmul(p[:], lhsT=condB[:, kk, :], rhs=w[:, kk, :],
                                                    start=(kk == 0), stop=False),
                     reads=[condB.b, w.b], writes=[p.b])
            k.op("pe", lambda e: e.matmul(p[:], lhsT=c["ones_f"][0:1, :], rhs=brow[0:1, j * 512:(j + 1) * 512],
                                          start=False, stop=True),
                 reads=[c["ones_f"].b, brow.b], writes=[p.b])
            k.op("act", lambda e: e.activation(out=modb[:, j * 512:(j + 1) * 512], in_=p[:], func=AF.Identity),
                 reads=[p.b], writes=[modb.b])
        for which, col in ((0, 1024), (1, 4096)):
            for hh in range(2):
                p = pm[hh]
                k.op("pe", lambda e: e.matmul(p[:], lhsT=c["ones_f"][0:1, :],
                                              rhs=nrow[0:1, which * 1024 + hh * 512: which * 1024 + (hh + 1) * 512],
                                              start=True, stop=True),
                     reads=[c["ones_f"].b, nrow.b], writes=[p.b])
                sl = modb[:, col + hh * 512: col + (hh + 1) * 512]
                k.op("dve", lambda e: e.scalar_tensor_tensor(out=sl, in0=sl, scalar=1.0, in1=p[:],
                                                             op0=ALU.add, op1=ALU.mult),
                     reads=[p.b], writes=[modb.b])
    k.barrier()


def emit_norm(cx, c, es, src_tile, modb, gcol, scol, dstT, tiles, tok0, router=None, rows_dst=None):
    k = cx.k
    NBUF = 4
    tiles = list(tiles)
    xin = [cx.sb("n_xin%d" % i, [128, D], F32, es) for i in range(NBUF)]
    junk = cx.sb("n_junk", [128, D], BF16, es)
    tmp = [cx.sb("n_tmp%d" % i, [128, D], F32, es) for i in range(NBUF)]
    st = [cx.sb("n_st%d" % i, [128, 4], F32, es) for i in range(NBUF)]
    if rows_dst is None:
        hrow = [cx.sb("n_hrow%d" % i, [128, D], BF16, es) for i in range(NBUF)]
        ptr = [cx.ps("n_ptr%d" % i, [128, 8, 128], BF16, es) for i in range(2)]
    if router is not None:
        hf = [cx.sb("n_hf%d" % i, [128, D], F32, es) for i in range(NBUF)]
        ptf = [[cx.ps("n_ptf", [128, 4, 128], F32, es) for i in range(2)] for half in range(2)]
        hTf = [cx.sb("n_hTf%d" % i, [128, 8, 128], F32, es) for i in range(2)]
        plog = [cx.ps("n_plog%d" % i, [128, 16], F32, es) for i in range(2)]

    def stA(n):
        i = tiles[n]
        x, s = xin[n % NBUF], st[n % NBUF]
        k.dma("sp", x[:], src_tile(i), reads=[src_tile.b], writes=[x.b])
        k.op("act", lambda e: e.activation(out=junk[:], in_=x[:], func=AF.Square, accum_out=s[:, 0:1]),
             reads=[x.b], writes=[junk.b, s.b])
        k.op("pool", lambda e: e.tensor_scalar(out=s[:, 1:2], in0=s[:, 0:1], scalar1=1.0 / D, scalar2=EPS,
                                               op0=ALU.mult, op1=ALU.add), writes=[s.b])
        k.op("act", lambda e: e.activation(out=s[:, 2:3], in_=s[:, 1:2], func=AF.Ln), writes=[s.b])
        k.op("act", lambda e: e.activation(out=s[:, 3:4], in_=s[:, 2:3], func=AF.Exp, scale=-0.5), writes=[s.b])

    def stB(n):
        i = tiles[n]
        x, s, t = xin[n % NBUF], st[n % NBUF], tmp[n % NBUF]
        k.op("dve", lambda e: e.scalar_tensor_tensor(out=t[:], in0=x[:], scalar=s[:, 3:4],
                                                     in1=modb[:, gcol:gcol + D], op0=ALU.mult, op1=ALU.mult),
             reads=[x.b, s.b, modb.b], writes=[t.b])
        if router is None:
            hr = hrow[n % NBUF]
            k.op("dve", lambda e: e.tensor_tensor(out=hr[:], in0=t[:], in1=modb[:, scol:scol + D], op=ALU.add),
                 reads=[t.b, modb.b], writes=[hr.b])
        else:
            h32 = hf[n % NBUF]
            k.op("dve", lambda e: e.tensor_tensor(out=h32[:], in0=t[:], in1=modb[:, scol:scol + D], op=ALU.add),
                 reads=[t.b, modb.b], writes=[h32.b])
            if rows_dst is not None:
                k.op("act", lambda e: e.activation(out=rows_dst[:, i, :], in_=h32[:], func=AF.Identity),
                     reads=[h32.b], writes=[rows_dst.b])
            else:
                hr = hrow[n % NBUF]
                k.op("act", lambda e: e.activation(out=hr[:], in_=h32[:], func=AF.Identity),
                     reads=[h32.b], writes=[hr.b])
        if rows_dst is None:
            p = ptr[n % 2]
            for kk in range(8):
                k.op("pe", lambda e: e.transpose(p[:, kk, :], hr[:, kk * 128:(kk + 1) * 128], c["id_b"][:]),
                     reads=[hr.b, c["id_b"].b], writes=[p.b])
        if router is not None:
            for half in range(2):
                pf = ptf[half][n % 2]
                for kk in range(4):
                    kc = half * 4 + kk
                    k.op("pe", lambda e: e.transpose(pf[:, kk, :], h32[:, kc * 128:(kc + 1) * 128], c["id_f"][:]),
                         reads=[h32.b, c["id_f"].b], writes=[pf.b])

    def stC(n):
        i = tiles[n]
        if rows_dst is None:
            p = ptr[n % 2]
            col = i * 128 - tok0
            k.op("dve", lambda e: e.tensor_copy(out=dstT[:, :, col:col + 128], in_=p[:]),
                 reads=[p.b], writes=[dstT.b])
        if router is not None:
            hT32 = hTf[n % 2]
            for half in range(2):
                pf = ptf[half][n % 2]
                k.op("act", lambda e: e.activation(out=hT32[:, half * 4:(half + 1) * 4, :], in_=pf[:],
                                                   func=AF.Identity), reads=[pf.b], writes=[hT32.b])
            pl = plog[n % 2]
            for kk in range(8):
                k.op("pe", lambda e: e.matmul(pl[:], lhsT=hT32[:, kk, :], rhs=router["rw"][:, kk, :],
                                              start=(kk == 0), stop=(kk == 7)),
                     reads=[hT32.b, router["rw"].b], writes=[pl.b])
            k.op("dve", lambda e: e.tensor_tensor(out=router["logits"][:, i, :], in0=pl[:], in1=router["rb"][:],
                                                  op=ALU.add),
                 reads=[pl.b, router["rb"].b], writes=[router["logits"].b])

    N = len(tiles)
    for step in range(N + 2):
        if step < N:
            stA(step)
        if 0 <= step - 1 < N:
            stB(step - 1)
        if 0 <= step - 2 < N:
            stC(step - 2)


def emit_router(cx, es, logits, comb, t0, t1, G=None, combg=None):
    k = cx.k
    n = t1 - t0
    mx = cx.sb("r_mx", [128, n], F32, es)
    u = cx.sb("r_u", [128, n, 4, 4], F32, es)
    ps_ = cx.sb("r_ps", [128, n, 4, 6], F32, es)
    sel = cx.sb("r_sel", [128, n, 4, 6], F32, es)
    msk = cx.sb("r_msk", [128, n, 4, 4], F32, es)
    rm = cx.sb("r_rm", [128, n], F32, es)
    lg = logits[:, t0:t1, :]
    uf = u[:].rearrange("p n g e -> p n (g e)")
    k.op("dve", lambda e: e.tensor_reduce(out=mx[:], in_=lg, axis=AX.X, op=ALU.max),
         reads=[logits.b], writes=[mx.b])
    k.op("dve", lambda e: e.tensor_tensor(out=uf, in0=lg, in1=mx[:].unsqueeze(2).to_broadcast([128, n, 16]),
                                          op=ALU.subtract),
         reads=[logits.b, mx.b], writes=[u.b])
    k.op("act", lambda e: e.activation(out=uf, in_=uf, func=AF.Exp), reads=[], writes=[u.b])
    k.op("dve", lambda e: e.tensor_tensor(out=ps_[:, :, :, 0:3], in0=u[:, :, :, 0:3], in1=u[:, :, :, 1:4], op=ALU.add),
         reads=[u.b], writes=[ps_.b])
    k.op("dve", lambda e: e.tensor_tensor(out=ps_[:, :, :, 3:5], in0=u[:, :, :, 0:2], in1=u[:, :, :, 2:4], op=ALU.add),
         reads=[u.b], writes=[ps_.b])
    k.op("dve", lambda e: e.tensor_tensor(out=ps_[:, :, :, 5:6], in0=u[:, :, :, 0:1], in1=u[:, :, :, 3:4], op=ALU.add),
         reads=[u.b], writes=[ps_.b])
    psf = ps_[:].rearrange("p n g e -> p n (g e)")
    k.op("dve", lambda e: e.tensor_reduce(out=mx[:], in_=psf, axis=AX.X, op=ALU.max),
         reads=[ps_.b], writes=[mx.b])
    k.op("dve", lambda e: e.tensor_tensor(out=sel[:].rearrange("p n g e -> p n (g e)"), in0=psf,
                                          in1=mx[:].unsqueeze(2).to_broadcast([128, n, 24]), op=ALU.is_ge),
         reads=[ps_.b, mx.b], writes=[sel.b])
    k.op("dve", lambda e: e.reciprocal(out=rm[:], in_=mx[:]), reads=[mx.b], writes=[rm.b])
    k.op("pool", lambda e: e.memset(msk[:], 0.0), writes=[msk.b])
    for (a0, a1, s0, s1) in ((0, 3, 0, 3), (1, 4, 0, 3), (0, 2, 3, 5), (2, 4, 3, 5), (0, 1, 5, 6), (3, 4, 5, 6)):
        k.op("dve", lambda e, a0=a0, a1=a1, s0=s0, s1=s1: e.tensor_tensor(
            out=msk[:, :, :, a0:a1], in0=msk[:, :, :, a0:a1], in1=sel[:, :, :, s0:s1], op=ALU.add),
            reads=[sel.b], writes=[msk.b])
    k.op("dve", lambda e: e.tensor_tensor(out=uf, in0=uf, in1=msk[:].rearrange("p n g e -> p n (g e)"), op=ALU.mult),
         reads=[msk.b], writes=[u.b])
    k.op("dve", lambda e: e.tensor_tensor(out=comb[:, t0:t1, :], in0=uf,
                                          in1=rm[:].unsqueeze(2).to_broadcast([128, n, 16]), op=ALU.mult),
         reads=[u.b, rm.b], writes=[comb.b])
    if G is None:
        return
    gh = cx.sb("r_gh", [128, n, 4], F32, es)
    npv = cx.sb("r_np", [128, n], F32, es)
    k.op("dve", lambda e: e.tensor_reduce(out=gh[:], in_=sel[:], axis=AX.X, op=ALU.max), reads=[sel.b], writes=[gh.b])
    k.op("dve", lambda e: e.tensor_copy(out=G[:, :, 0], in_=gh[:, :, 0]), reads=[gh.b], writes=[G.b])
    k.op("dve", lambda e: e.tensor_scalar(out=npv[:], in0=gh[:, :, 0], scalar1=-1.0, scalar2=1.0, op0=ALU.mult,
                                          op1=ALU.add), reads=[gh.b], writes=[npv.b])
    for g in range(1, 4):
        k.op("dve", lambda e: e.tensor_tensor(out=G[:, :, g], in0=gh[:, :, g], in1=npv[:], op=ALU.mult),
             reads=[gh.b, npv.b], writes=[G.b])
        if g < 3:
            k.op("dve", lambda e: e.tensor_tensor(out=npv[:], in0=npv[:], in1=G[:, :, g], op=ALU.subtract),
                 reads=[G.b], writes=[npv.b])
    prod = cx.sb("r_prod", [128, n, 4, 4], F32, es)
    k.op("dve", lambda e: e.tensor_tensor(out=prod[:], in0=comb[:, t0:t1, :].rearrange("p n (g j) -> p n g j", g=4),
                                          in1=G[:].unsqueeze(3).to_broadcast([128, n, 4, 4]), op=ALU.mult),
         reads=[comb.b, G.b], writes=[prod.b])
    k.op("dve", lambda e: e.tensor_reduce(out=combg[:], in_=prod[:].rearrange("p n g j -> p n j g"), axis=AX.X,
                                          op=ALU.add), reads=[prod.b], writes=[combg.b])


def emit_moe(cx, es, L, h2T, comb, acc, t0, ntile, w_gate, w_up, w_down):
    k = cx.k
    wg = [cx.sb("m_wg%d" % i, [128, 8, DFF], BF16, es) for i in range(2)]
    wu = [cx.sb("m_wu%d" % i, [128, 8, DFF], BF16, es) for i in range(2)]
    wd = [cx.sb("m_wd%d" % i, [128, 4, D], BF16, es) for i in range(2)]
    sg = [cx.sb("m_sg%d" % i, [128, 512], F32, es) for i in range(2)]
    hid = [cx.sb("m_hid%d" % i, [128, 4, 512], BF16, es) for i in range(2)]
    pg = [cx.ps("m_pg%d" % i, [128, 512], F32, es) for i in range(2)]
    pu = [cx.ps("m_pu%d" % i, [128, 512], F32, es) for i in range(2)]
    pd = [cx.ps("m_pd%d" % i, [128, 512], F32, es) for i in range(2)]
    k.op("pool", lambda e: e.memset(acc[:], 0.0), writes=[acc.b])
    ngrp = ntile // 4

    def load(e):
        i = e % 2
        k.dma("pool", wg[i][:], w_gate[L, e].rearrange("(k p) f -> p k f", p=128), writes=[wg[i].b])
        k.dma("pool", wu[i][:], w_up[L, e].rearrange("(k p) f -> p k f", p=128), writes=[wu[i].b])
        k.dma("pool", wd[i][:], w_down[L, e].rearrange("(k p) f -> p k f", p=128), writes=[wd[i].b])

    load(0)
    it = 0
    for e in range(NEXP):
        if e + 1 < NEXP:
            load(e + 1)
        g_, u_, d_ = wg[e % 2], wu[e % 2], wd[e % 2]
        for tg in range(ngrp):
            hd = hid[it % 2]
            it += 1
            c0 = tg * 512
            for fc in range(4):
                pG = pg[fc % 2]
                pU = pu[fc % 2]
                s_ = sg[fc % 2]
                for kk in range(8):
                    k.op("pe", lambda en, kk=kk: en.matmul(pG[:], lhsT=g_[:, kk, fc * 128:(fc + 1) * 128],
                                                          rhs=h2T[:, kk, c0:c0 + 512], start=(kk == 0), stop=(kk == 7)),
                         reads=[g_.b, h2T.b], writes=[pG.b])
                for kk in range(8):
                    k.op("pe", lambda en, kk=kk: en.matmul(pU[:], lhsT=u_[:, kk, fc * 128:(fc + 1) * 128],
                                                          rhs=h2T[:, kk, c0:c0 + 512], start=(kk == 0), stop=(kk == 7)),
                         reads=[u_.b, h2T.b], writes=[pU.b])
                k.op("act", lambda en: en.activation(out=s_[:], in_=pG[:], func=AF.Silu),
                     reads=[pG.b], writes=[s_.b])
                k.op("dve", lambda en: en.tensor_tensor(out=hd[:, fc, :], in0=pU[:], in1=s_[:], op=ALU.mult),
                     reads=[pU.b, s_.b], writes=[hd.b])
            for tt in range(4):
                ti = tg * 4 + tt
                for dh in range(2):
                    pD = pd[dh]
                    for fc in range(4):
                        k.op("pe", lambda en, fc=fc: en.matmul(pD[:], lhsT=hd[:, fc, tt * 128:(tt + 1) * 128],
                                                              rhs=d_[:, fc, dh * 512:(dh + 1) * 512],
                                                              start=(fc == 0), stop=(fc == 3)),
                             reads=[hd.b, d_.b], writes=[pD.b])
                    a = acc[:, ti, dh * 512:(dh + 1) * 512]
                    k.op("dve", lambda en: en.scalar_tensor_tensor(out=a, in0=pD[:], scalar=comb[:, t0 + ti, e:e + 1],
                                                                   in1=a, op0=ALU.mult, op1=ALU.add),
                         reads=[pD.b, comb.b], writes=[acc.b])


def emit_ffn(cx, c, L, modb, xsrc, xdst, router_w, router_b, w_gate, w_up, w_down, nhalf=2):
    k = cx.k
    NT = cx.NT
    per = NT // nhalf
    with contextlib.ExitStack() as es:
        rw = cx.sb("f_rw", [128, 8, 16], F32, es)
        rb = cx.sb("f_rb", [128, 16], F32, es)
        rb1 = cx.sb("f_rb1", [1, 16], F32, es)
        logits = cx.sb("f_logits", [128, NT, 16], F32, es)
        comb = cx.sb("f_comb", [128, NT, 16], F32, es)
        h2T = cx.sb("f_h2T", [128, 8, per * 128], BF16, es)
        acc = cx.sb("f_acc", [128, per, D], F32, es)
        k.dma("sp", rw[:], router_w.rearrange("(k p) e -> p k e", p=128), writes=[rw.b])
        k.dma("sp", rb1[:], router_b, writes=[rb1.b])
        with contextlib.ExitStack() as es2:
            prb = cx.ps("f_prb", [128, 16], F32, es2)
            k.op("pe", lambda e: e.matmul(prb[:], lhsT=c["ones_f"][0:1, :], rhs=rb1[0:1, :], start=True, stop=True),
                 reads=[c["ones_f"].b, rb1.b], writes=[prb.b])
            k.op("act", lambda e: e.activation(out=rb[:], in_=prb[:], func=AF.Identity), reads=[prb.b], writes=[rb.b])
            k.barrier()
        for half in range(nhalf):
            t0 = half * per
            with contextlib.ExitStack() as es2:
                emit_norm(cx, c, es2, xsrc, modb, 4096, 3072, h2T, range(t0, t0 + per), t0 * 128,
                          router=dict(rw=rw, rb=rb, logits=logits))
                emit_router(cx, es2, logits, comb, t0, t0 + per)
                k.barrier()
            with contextlib.ExitStack() as es2:
                emit_moe(cx, es2, L, h2T, comb, acc, t0, per, w_gate, w_up, w_down)
                k.barrier()
            with contextlib.ExitStack() as es2:
                xin = [cx.sb("o_xin%d" % i, [128, D], F32, es2) for i in range(2)]
                xo = [cx.sb("o_xo%d" % i, [128, D], F32, es2) for i in range(2)]
                for n in range(per):
                    i = t0 + n
                    x = xin[n % 2]
                    o = xo[n % 2]
                    k.dma("sp", x[:], xsrc(i), reads=[xsrc.b], writes=[x.b])
                    k.op("dve", lambda e: e.tensor_tensor(out=o[:], in0=acc[:, n, :], in1=modb[:, 5120:6144],
                                                          op=ALU.mult), reads=[acc.b, modb.b], writes=[o.b])
                    k.op("pool", lambda e: e.tensor_tensor(out=o[:], in0=o[:], in1=x[:], op=ALU.add),
                         reads=[x.b], writes=[o.b])
                    k.dma("sp", xdst(i), o[:], reads=[o.b], writes=[xdst.b])
                k.barrier()


def load_w(cx, wt, w_in, L, c0, ncol):
    cx.k.dma("pool", wt[:, :, 0:ncol], w_in[L, :, c0:c0 + ncol].rearrange("(k p) c -> p k c", p=128),
             writes=[wt.b])


def proj_featT(cx, hT, wt, m, pbanks, evac):
    k = cx.k
    for tg in range(cx.S // 512):
        p = pbanks[tg % len(pbanks)]
        for kk in range(8):
            k.op("pe", lambda e: e.matmul(p[0:m, :], lhsT=wt[:, kk, 0:m], rhs=hT[:, kk, tg * 512:(tg + 1) * 512],
                                          start=(kk == 0), stop=(kk == 7)),
                 reads=[wt.b, hT.b], writes=[p.b])
        evac(tg, p)


class QKNorm:
    def __init__(self, cx, c, es):
        self.cx, self.c = cx, c
        self.sq = [cx.sb("qn_sq%d" % i, [64, 512], BF16, es) for i in range(2)]
        self.r = [cx.sb("qn_r%d" % i, [64, 512], F32, es) for i in range(2)]
        self.pss = [cx.ps("qn_pss%d" % i, [64, 512], F32, es) for i in range(2)]
        self.n = 0

    def __call__(self, p, gain, dst_ap, dst_buf):
        k, c = self.cx.k, self.c
        sq, r, pss = self.sq[self.n % 2], self.r[self.n % 2], self.pss[self.n % 2]
        self.n += 1
        k.op("act", lambda e: e.activation(out=sq[:], in_=p[0:64, :], func=AF.Square), reads=[p.b], writes=[sq.b])
        k.op("pe", lambda e: e.matmul(pss[:], lhsT=c["ones_b"][0:64, 0:64], rhs=sq[:], start=True, stop=True),
             reads=[sq.b, c["ones_b"].b], writes=[pss.b])
        k.op("act", lambda e: e.activation(out=r[:], in_=pss[:], func=AF.Ln, scale=1.0 / HD, bias=c["eps"][0:64, :]),
             reads=[pss.b, c["eps"].b], writes=[r.b])
        k.op("act", lambda e: e.activation(out=r[:], in_=r[:], func=AF.Exp, scale=-0.5), writes=[r.b])
        k.op("dve", lambda e: e.scalar_tensor_tensor(out=dst_ap, in0=p[0:64, :], scalar=gain[:], in1=r[:],
                                                     op0=ALU.mult, op1=ALU.mult),
             reads=[p.b, r.b, gain.b], writes=[dst_buf])


def load_gain(cx, es, name, src, L, scale):
    k = cx.k
    g = cx.sb(name, [64, 1], F32, es)
    k.dma("sp", g[:], src[L:L + 1, :].rearrange("o e -> e o"), writes=[g.b])
    if scale != 1.0:
        k.op("dve", lambda e: e.tensor_scalar(out=g[:], in0=g[:], scalar1=scale, scalar2=None, op0=ALU.mult),
             writes=[g.b])
    return g


def emit_fox(cx, c, L, hT, w_in, b_forget, q_gain, k_gain, mixedT):
    k = cx.k
    S, NT, NG = cx.S, cx.NT, cx.S // 512
    with contextlib.ExitStack() as es:
        vaug = cx.sb("fx_vaug", [128, NT, 4, 65], BF16, es)
        cs3 = cx.sb("fx_cs3", [4, 3, S], BF16, es)
        qa = [cx.sb("fx_qa%d" % i, [70, S], BF16, es) for i in range(2)]
        ka = [cx.sb("fx_ka%d" % i, [70, S], BF16, es) for i in range(2)]
        gq = load_gain(cx, es, "fx_gq", q_gain, L, 0.125)
        gk = load_gain(cx, es, "fx_gk", k_gain, L, 1.0)
        with contextlib.ExitStack() as es2:
            wv = cx.sb("fx_wv", [128, 8, 256], BF16, es2)
            wf = cx.sb("fx_wf", [128, 8, 4], BF16, es2)
            nb = cx.sb("fx_nb", [4, 1], F32, es2)
            A = cx.sb("fx_A", [4, S], F32, es2)
            B = cx.sb("fx_B", [4, S], F32, es2)
            pv = [cx.ps("fx_pv%d" % i, [128, 512], F32, es2) for i in range(2)]
            pf = [cx.ps("fx_pf%d" % i, [128, 512], F32, es2) for i in range(2)]
            load_w(cx, wv, w_in, L, 512, 256)
            load_w(cx, wf, w_in, L, 1024, 4)
            k.dma("sp", nb[:], b_forget[L:L + 1, :].rearrange("o h -> h o"), writes=[nb.b])
            k.op("dve", lambda e: e.tensor_scalar(out=nb[:], in0=nb[:], scalar1=-1.0, scalar2=None, op0=ALU.mult),
                 writes=[nb.b])
            k.op("pool", lambda e: e.memset(vaug[:], 1.0), writes=[vaug.b])
            for i in range(NT):
                p = pv[i % 2]
                for kk in range(8):
                    k.op("pe", lambda e: e.matmul(p[:, 0:256], lhsT=hT[:, kk, i * 128:(i + 1) * 128], rhs=wv[:, kk, :],
                                                  start=(kk == 0), stop=(kk == 7)),
                         reads=[hT.b, wv.b], writes=[p.b])
                k.op("act", lambda e: e.activation(out=vaug[:, i, :, 0:64],
                                                   in_=p[:, 0:256].rearrange("p (h e) -> p h e", h=4),
                                                   func=AF.Identity), reads=[p.b], writes=[vaug.b])
            for tg in range(NG):
                p = pf[tg % 2]
                for kk in range(8):
                    k.op("pe", lambda e: e.matmul(p[0:4, :], lhsT=wf[:, kk, :], rhs=hT[:, kk, tg * 512:(tg + 1) * 512],
                                                  start=(kk == 0), stop=(kk == 7)),
                         reads=[hT.b, wf.b], writes=[p.b])
                k.op("act", lambda e: e.activation(out=A[:, tg * 512:(tg + 1) * 512], in_=p[0:4, :], func=AF.Exp,
                                                   scale=-1.0, bias=nb[:]), reads=[p.b, nb.b], writes=[A.b])
            k.op("act", lambda e: e.activation(out=A[:], in_=A[:], func=AF.Ln, bias=c["one"][0:4, :]),
                 reads=[c["one"].b], writes=[A.b])
            k.op("dve", lambda e: e.tensor_tensor_scan(out=B[:], data0=A[:], data1=A[:], initial=0.0,
                                                       op0=ALU.add, op1=ALU.max), reads=[A.b], writes=[B.b])
            k.op("dve", lambda e: e.tensor_copy(out=cs3[:, 0, :], in_=B[:]), reads=[B.b], writes=[cs3.b])
            k.op("dve", lambda e: e.tensor_tensor(out=A[:], in0=B[:], in1=cs3[:, 0, :], op=ALU.subtract),
                 reads=[B.b, cs3.b], writes=[A.b])
            k.op("dve", lambda e: e.tensor_copy(out=cs3[:, 1, :], in_=A[:]), reads=[A.b], writes=[cs3.b])
            k.op("dve", lambda e: e.tensor_tensor(out=A[:], in0=A[:], in1=cs3[:, 1, :], op=ALU.subtract),
                 reads=[cs3.b], writes=[A.b])
            k.op("dve", lambda e: e.tensor_copy(out=cs3[:, 2, :], in_=A[:]), reads=[A.b], writes=[cs3.b])
            k.barrier()
        with contextlib.ExitStack() as es2:
            wq = [cx.sb("fx_wq%d" % i, [128, 8, 64], BF16, es2) for i in range(2)]
            wk = [cx.sb("fx_wk%d" % i, [128, 8, 64], BF16, es2) for i in range(2)]
            wg = [cx.sb("fx_wg%d" % i, [128, 8, 64], BF16, es2) for i in range(2)]
            lanes = []
            for ln in range(2):
                lanes.append(dict(
                    pt=[cx.sb("fx_pt", [128, 512], BF16, es2) for i in range(3)],
                    rl=cx.sb("fx_rl", [128, 512], F32, es2),
                    osb=cx.sb("fx_osb", [64, 512], F32, es2),
                    eg=cx.sb("fx_eg", [64, 512], F32, es2),
                    ob=[cx.sb("fx_ob", [64, 512], BF16, es2) for i in range(2)],
                    ps=[cx.ps("fx_ps", [128, 512], F32, es2) for i in range(2)],
                    po=cx.ps("fx_po", [128, 512], F32, es2),
                    pm=cx.ps("fx_pm", [128, 512], F32, es2)))
            sq = [cx.sb("qn_sq", [64, 512], BF16, es2) for i in range(2)]
            rr = [cx.sb("qn_r", [64, 512], F32, es2) for i in range(2)]

            def project(h, QA, KA, Wq, Wk, R):
                n = 0
                for (W, dst, gain) in ((Wq, QA, gq), (Wk, KA, gk)):
                    for tg in range(NG):
                        p, pss = R["ps"][tg % 2], (R["po"], R["pm"])[tg % 2]
                        s_, r_ = sq[n % 2], rr[n % 2]
                        n += 1
                        for kk in range(8):
                            k.op("pe", lambda e: e.matmul(p[0:64, :], lhsT=W[:, kk, :],
                                                          rhs=hT[:, kk, tg * 512:(tg + 1) * 512],
                                                          start=(kk == 0), stop=(kk == 7)),
                                 reads=[W.b, hT.b], writes=[p.b])
                        k.op("act", lambda e: e.activation(out=s_[:], in_=p[0:64, :], func=AF.Square),
                             reads=[p.b], writes=[s_.b])
                        k.op("pe", lambda e: e.matmul(pss[0:64, :], lhsT=c["ones_b"][0:64, 0:64], rhs=s_[:],
                                                      start=True, stop=True), reads=[s_.b, c["ones_b"].b],
                             writes=[pss.b])
                        k.op("act", lambda e: e.activation(out=r_[:], in_=pss[0:64, :], func=AF.Ln, scale=1.0 / HD,
                                                           bias=c["eps"][0:64, :]), reads=[pss.b, c["eps"].b],
                             writes=[r_.b])
                        k.op("act", lambda e: e.activation(out=r_[:], in_=r_[:], func=AF.Exp, scale=-0.5),
                             writes=[r_.b])
                        k.op("dve", lambda e: e.scalar_tensor_tensor(out=dst[0:64, tg * 512:(tg + 1) * 512],
                                                                     in0=p[0:64, :], scalar=gain[:], in1=r_[:],
                                                                     op0=ALU.mult, op1=ALU.mult),
                             reads=[p.b, r_.b, gain.b], writes=[dst.b])

            def attn_gen(h, QA, KA, Wg, R):
                pt, rl, osb, eg, pO, pG = R["pt"], R["rl"], R["osb"], R["eg"], R["po"], R["pm"]
                for g in range(NG):
                    c0 = g * 512
                    nkb = 4 * g + 4
                    steps = [(kb, 128 * max(kb - 4 * g, 0), kb - 4 * g >= 0) for kb in range(nkb)]

                    def stA(j):
                        kb, lo, dg = steps[j]
                        pS = R["ps"][j % 2]
                        k.op("pe", lambda e: e.matmul(pS[:, lo:512], lhsT=KA[0:70, kb * 128:(kb + 1) * 128],
                                                      rhs=QA[0:70, c0 + lo:c0 + 512], start=True, stop=True),
                             reads=[KA.b, QA.b], writes=[pS.b])

                    def stB(j):
                        kb, lo, dg = steps[j]
                        pS, P = R["ps"][j % 2], pt[j % 3]
                        k.op("act", lambda e: e.activation(out=P[:, lo:512], in_=pS[:, lo:512], func=AF.Exp),
                             reads=[pS.b], writes=[P.b])
                        if dg:
                            k.op("pool", lambda e: e.affine_select(out=P[:, lo:lo + 128], in_=P[:, lo:lo + 128],
                                                                   pattern=[[1, 128]], compare_op=ALU.is_ge, fill=0.0,
                                                                   base=0, channel_multiplier=-1), writes=[P.b])

                    def stC(j):
                        kb, lo, dg = steps[j]
                        P = pt[j % 3]
                        k.op("pe", lambda e: e.matmul(pO[0:65, lo:512], lhsT=vaug[:, kb, h, :], rhs=P[:, lo:512],
                                                      start=(j == 0), stop=(j == nkb - 1)),
                             reads=[vaug.b, P.b], writes=[pO.b])

                    stA(0)
                    stA(1)
                    for j in range(nkb):
                        stB(j)
                        stC(j)
                        if j + 2 < nkb:
                            stA(j + 2)
                        if j % 2 == 1:
                            yield
                    O = R["ob"][g % 2]
                    k.op("act", lambda e: e.activation(out=rl[64:65, :], in_=pO[64:65, :], func=AF.Ln),
                         reads=[pO.b], writes=[rl.b])
                    k.op("act", lambda e: e.activation(out=rl[64:65, :], in_=rl[64:65, :], func=AF.Exp, scale=-1.0),
                         writes=[rl.b])
                    k.op("act", lambda e: e.activation(out=osb[:], in_=pO[0:64, :], func=AF.Identity),
                         reads=[pO.b], writes=[osb.b])
                    pB = R["ps"][0]
                    k.op("pe", lambda e: e.matmul(pB[0:64, :], lhsT=c["ones_f"][64:65, 0:64], rhs=rl[64:65, :],
                                                  start=True, stop=True), reads=[rl.b, c["ones_f"].b], writes=[pB.b])
                    for kk in range(8):
                        k.op("pe", lambda e: e.matmul(pG[0:64, :], lhsT=Wg[:, kk, :], rhs=hT[:, kk, c0:c0 + 512],
                                                      start=(kk == 0), stop=(kk == 7)),
                             reads=[Wg.b, hT.b], writes=[pG.b])
                    k.op("act", lambda e: e.activation(out=eg[:], in_=pG[0:64, :], func=AF.Exp, scale=-1.0),
                         reads=[pG.b], writes=[eg.b])
                    k.op("act", lambda e: e.activation(out=eg[:], in_=eg[:], func=AF.Ln, bias=c["one"][0:64, :]),
                         reads=[c["one"].b], writes=[eg.b])
                    k.op("act", lambda e: e.activation(out=eg[:], in_=eg[:], func=AF.Exp, scale=-1.0), writes=[eg.b])
                    k.op("dve", lambda e: e.tensor_tensor(out=osb[:], in0=osb[:], in1=pB[0:64, :], op=ALU.mult),
                         reads=[pB.b], writes=[osb.b])
                    k.op("dve", lambda e: e.tensor_tensor(out=O[:], in0=osb[:], in1=eg[:], op=ALU.mult),
                         reads=[osb.b, eg.b], writes=[O.b])
                    k.dma("sp", mixedT[64 * h:64 * h + 64, c0:c0 + 512], O[:], reads=[O.b], writes=[mixedT.b])
                    yield

            for pr_ in range(2):
                gens = []
                for ln in range(2):
                    h = 2 * pr_ + ln
                    QA, KA = qa[ln], ka[ln]
                    load_w(cx, wq[ln], w_in, L, 64 * h, 64)
                    load_w(cx, wk[ln], w_in, L, 256 + 64 * h, 64)
                    load_w(cx, wg[ln], w_in, L, 768 + 64 * h, 64)
                    k.op("pool", lambda e: e.memset(QA[64:70, :], 1.0), writes=[QA.b])
                    k.op("pool", lambda e: e.memset(KA[64:70, :], 1.0), writes=[KA.b])
                    for j in range(3):
                        k.dma("sp", QA[64 + j:65 + j, :], cs3[h:h + 1, j, :], reads=[cs3.b], writes=[QA.b])
                        k.dma("sp", KA[67 + j:68 + j, :], cs3[h:h + 1, j, :], reads=[cs3.b], writes=[KA.b])
                    k.op("dve", lambda e: e.tensor_scalar(out=QA[64:67, :], in0=QA[64:67, :], scalar1=-1.0,
                                                          scalar2=None, op0=ALU.mult), writes=[QA.b])
                    project(h, QA, KA, wq[ln], wk[ln], lanes[ln])
                    gens.append(attn_gen(h, QA, KA, wg[ln], lanes[ln]))
                run_lanes(gens)
            k.barrier()


def run_lanes(gens):
    gens = list(gens)
    while gens:
        for g in list(gens):
            try:
                next(g)
            except StopIteration:
                gens.remove(g)


def emit_sb(cx, c, L, hT, w_in, mixedT):
    k = cx.k
    S, NT, NG = cx.S, cx.NT, cx.S // 512
    QOFF, KOFF, VOFF, MOFF = 2180, 2564, 2948, 640
    NL = 2
    with contextlib.ExitStack() as es:
        vs = cx.sb("sb_v", [128, NT, 6, 64], BF16, es)
        with contextlib.ExitStack() as es2:
            wv = cx.sb("sb_wv", [128, 8, 384], BF16, es2)
            pv = [cx.ps("sb_pv%d" % i, [128, 512], F32, es2) for i in range(2)]
            load_w(cx, wv, w_in, L, VOFF, 384)
            for i in range(NT):
                p = pv[i % 2]
                for kk in range(8):
                    k.op("pe", lambda e: e.matmul(p[:, 0:384], lhsT=hT[:, kk, i * 128:(i + 1) * 128], rhs=wv[:, kk, :],
                                                  start=(kk == 0), stop=(kk == 7)),
                         reads=[hT.b, wv.b], writes=[p.b])
                k.op("act", lambda e: e.activation(out=vs[:, i, :, :],
                                                   in_=p[:, 0:384].rearrange("p (h e) -> p h e", h=6),
                                                   func=AF.Identity), reads=[p.b], writes=[vs.b])
            k.barrier()
        with contextlib.ExitStack() as es2:
            CH = 16
            wq = cx.sb("sb_wq", [128, 8, 128], BF16, es2)
            wk = cx.sb("sb_wk", [128, 8, 128], BF16, es2)
            Kp = cx.sb("sb_kp", [128, S], BF16, es2)
            NKp = cx.sb("sb_nkp", [128, S], BF16, es2)
            Qp = [cx.sb("sb_q", [128, S], BF16, es2) for i in range(2)]
            k.op("pool", lambda e: e.memset(Qp[0][64:128, :], 0.0), writes=[Qp[0].b])
            k.op("pool", lambda e: e.memset(Qp[1][0:64, :], 0.0), writes=[Qp[1].b])
            lanes = []
            for ln in range(NL):
                R = dict(
                    SP=[cx.sb("sb_SP", [128, 512], BF16, es2) for i in range(CH)],
                    A=[cx.sb("sb_A", [128, 512], BF16, es2) for i in range(2)],
                    acc=[cx.sb("sb_acc", [128, 512], BF16, es2) for i in range(2)],
                    ob=[cx.sb("sb_ob", [128, 512], BF16, es2) for i in range(2)],
                    pp=[cx.ps("sb_pp", [128, 512], F32, es2) for i in range(2)],
                    po=cx.ps("sb_po", [128, 512], F32, es2))
                lanes.append(R)

            def head_gen(R, h, Q, half):
                pO = R["po"]
                hs = slice(64 * half, 64 * half + 64)
                Vp = lambda kb: vs[:, kb, 2 * (h // 2):2 * (h // 2) + 2, :].rearrange("p h e -> p (h e)")
                steps = []
                for g in range(NG):
                    nkb = 4 * g + 4
                    for n_, kb in enumerate(range(nkb - 1, -1, -1)):
                        steps.append((g, kb, 128 * max(kb - 4 * g, 0), kb - 4 * g >= 0, n_ == 0, n_ == nkb - 1))
                ns = len(steps)

                def stZ(j):
                    g, kb, lo, dg, first, last = steps[j]
                    c0 = g * 512
                    pZ = R["pp"][j % 2]
                    k.op("pe", lambda e: e.matmul(pZ[:, lo:512], lhsT=Kp[:, kb * 128:(kb + 1) * 128],
                                                  rhs=Q[:, c0 + lo:c0 + 512], start=True, stop=True),
                         reads=[Kp.b, Q.b], writes=[pZ.b])

                def stS(j):
                    g, kb, lo, dg, first, last = steps[j]
                    pZ, SP, AC = R["pp"][j % 2], R["SP"][j % CH], R["acc"][g % 2]
                    if cx.dbg.get("nosoftplus"):
                        k.op("act", lambda e: e.activation(out=SP[:, lo:512], in_=pZ[:, lo:512], func=AF.Exp),
                             reads=[pZ.b], writes=[SP.b])
                        k.op("act", lambda e: e.activation(out=SP[:, lo:512], in_=SP[:, lo:512], func=AF.Ln,
                                                           bias=c["one"][:]), reads=[c["one"].b], writes=[SP.b])
                    else:
                        k.op("act", lambda e: e.activation(out=SP[:, lo:512], in_=pZ[:, lo:512], func=AF.Softplus),
                             reads=[pZ.b], writes=[SP.b])
                    if dg:
                        k.op("pool", lambda e: e.affine_select(out=SP[:, lo:lo + 128], in_=SP[:, lo:lo + 128],
                                                               pattern=[[1, 128]], compare_op=ALU.is_gt, fill=0.0,
                                                               base=0, channel_multiplier=-1), writes=[SP.b])

                def stC(j):
                    g, kb, lo, dg, first, last = steps[j]
                    c0 = g * 512
                    pR, SP, AC = R["pp"][j % 2], R["SP"][j % CH], R["acc"][g % 2]
                    k.op("pe", lambda e: e.matmul(pR[:, lo:512], lhsT=NKp[:, kb * 128:(kb + 1) * 128],
                                                  rhs=Q[:, c0 + lo:c0 + 512], start=True, stop=False),
                         reads=[NKp.b, Q.b], writes=[pR.b])
                    k.op("pe", lambda e: e.matmul(pR[:, lo:512], lhsT=c["tri_ge"][:], rhs=SP[:, lo:512],
                                                  start=False, stop=first), reads=[SP.b, c["tri_ge"].b],
                         writes=[pR.b])
                    if not first:
                        k.op("pe", lambda e: e.matmul(pR[:, lo:512], lhsT=c["ones_b"][:], rhs=AC[:, lo:512],
                                                      start=False, stop=True), reads=[AC.b, c["ones_b"].b],
                             writes=[pR.b])
                    if first:
                        k.op("pool", lambda e: e.memset(AC[:], 0.0), writes=[AC.b])
                    if not last:
                        k.op("dve", lambda e: e.tensor_tensor(out=AC[:, lo:512], in0=AC[:, lo:512],
                                                              in1=SP[:, lo:512], op=ALU.add),
                             reads=[SP.b], writes=[AC.b])

                def stD(j):
                    g, kb, lo, dg, first, last = steps[j]
                    pR, A_ = R["pp"][j % 2], R["A"][j % 2]
                    k.op("act", lambda e: e.activation(out=A_[:, lo:512], in_=pR[:, lo:512], func=AF.Exp,
                                                       scale=-1.0), reads=[pR.b], writes=[A_.b])
                    if dg:
                        k.op("pool", lambda e: e.affine_select(out=A_[:, lo:lo + 128], in_=A_[:, lo:lo + 128],
                                                               pattern=[[1, 128]], compare_op=ALU.is_gt, fill=0.0,
                                                               base=0, channel_multiplier=-1), writes=[A_.b])

                def stE(j):
                    g, kb, lo, dg, first, last = steps[j]
                    A_ = R["A"][j % 2]
                    if first:
                        k.op("pe", lambda e: e.matmul(pO[:, :], lhsT=c["zeros_b"][:, 0:128], rhs=c["zeros_b"][:],
                                                      start=True, stop=False), reads=[c["zeros_b"].b],
                             writes=[pO.b])
                    k.op("pe", lambda e: e.matmul(pO[:, lo:512], lhsT=Vp(kb), rhs=A_[:, lo:512],
                                                  start=False, stop=last), reads=[vs.b, A_.b], writes=[pO.b])
                    if last:
                        O = R["ob"][g % 2]
                        k.op("dve", lambda e: e.tensor_copy(out=O[hs, :], in_=pO[hs, :]),
                             reads=[pO.b], writes=[O.b])
                        k.dma("sp", mixedT[MOFF + 64 * h:MOFF + 64 * h + 64, g * 512:(g + 1) * 512], O[hs, :],
                              reads=[O.b], writes=[mixedT.b])

                for j0 in range(0, ns, CH):
                    j1 = min(ns, j0 + CH)
                    stZ(j0)
                    for j in range(j0, j1):
                        if j + 1 < j1:
                            stZ(j + 1)
                        stS(j)
                        yield
                    stC(j0)
                    for j in range(j0, j1):
                        stD(j)
                        if j + 1 < j1:
                            stC(j + 1)
                        stE(j)
                        yield

            for pr_ in range(3):
                load_w(cx, wq, w_in, L, QOFF + 128 * pr_, 128)
                load_w(cx, wk, w_in, L, KOFF + 128 * pr_, 128)
                for tg in range(NG):
                    cs = slice(tg * 512, (tg + 1) * 512)
                    p = lanes[0]["pp"][tg % 2]
                    for kk in range(8):
                        k.op("pe", lambda e: e.matmul(p[:], lhsT=wq[:, kk, :], rhs=hT[:, kk, cs],
                                                      start=(kk == 0), stop=(kk == 7)),
                             reads=[wq.b, hT.b], writes=[p.b])
                    k.op("act", lambda e: e.activation(out=Qp[0][0:64, cs], in_=p[0:64, :], func=AF.Identity,
                                                       scale=0.125), reads=[p.b], writes=[Qp[0].b])
                    k.op("dve", lambda e: e.tensor_scalar(out=Qp[1][64:128, cs], in0=p[64:128, :], scalar1=0.125,
                                                          scalar2=None, op0=ALU.mult), reads=[p.b], writes=[Qp[1].b])
                    p = lanes[1]["pp"][tg % 2]
                    for kk in range(8):
                        k.op("pe", lambda e: e.matmul(p[:], lhsT=wk[:, kk, :], rhs=hT[:, kk, cs],
                                                      start=(kk == 0), stop=(kk == 7)),
                             reads=[wk.b, hT.b], writes=[p.b])
                    k.op("act", lambda e: e.activation(out=Kp[:, cs], in_=p[:], func=AF.Identity),
                         reads=[p.b], writes=[Kp.b])
                    k.op("act", lambda e: e.activation(out=NKp[:, cs], in_=p[:], func=AF.Identity, scale=-1.0),
                         reads=[p.b], writes=[NKp.b])
                run_lanes([head_gen(lanes[ln], 2 * pr_ + ln, Qp[ln], ln) for ln in range(NL)])
            k.barrier()


DIL_PATTERNS = ((128, 1), (512, 4), (2048, 16))


def emit_dil(cx, c, L, hT, w_in, q_gain, k_gain, tb_gather, tb_mask, mixedT):
    k = cx.k
    S, NT, NG = cx.S, cx.NT, cx.S // 512
    QOFF, KOFF, VOFF, MOFF = 1028, 1412, 1796, 256
    with contextlib.ExitStack() as es:
        tbias = cx.sb("tbias", [128, 6, 3, 2, 128], BF16, es)
        with contextlib.ExitStack() as es2:
            tg32 = cx.sb("tg32", [128, 6, 768], F32, es2)
            tm32 = cx.sb("tm32", [128, 768], F32, es2)
            k.dma("sp", tg32[:], tb_gather, writes=[tg32.b])
            k.dma("sp", tm32[:], tb_mask, writes=[tm32.b])
            for h in range(6):
                k.op("dve", lambda e: e.tensor_tensor(out=tbias[:, h].rearrange("p a b q -> p (a b q)"),
                                                      in0=tg32[:, h, :], in1=tm32[:], op=ALU.add),
                     reads=[tg32.b, tm32.b], writes=[tbias.b])
            k.barrier()
        gq = cx.sb("dl_gq", [128, 1], F32, es)
        gk = cx.sb("dl_gk", [128, 1], F32, es)
        for half in range(2):
            k.dma("sp", gq[64 * half:64 * half + 64, :], q_gain[L:L + 1, :].rearrange("o e -> e o"), writes=[gq.b])
            k.dma("sp", gk[64 * half:64 * half + 64, :], k_gain[L:L + 1, :].rearrange("o e -> e o"), writes=[gk.b])
        k.op("dve", lambda e: e.tensor_scalar(out=gq[:], in0=gq[:], scalar1=0.125, scalar2=None, op0=ALU.mult),
             writes=[gq.b])
        bd = cx.sb("dl_bd", [128, 128], BF16, es)
        k.op("pool", lambda e: e.memset(bd[:], 0.0), writes=[bd.b])
        k.op("pool", lambda e: e.memset(bd[0:64, 0:64], 1.0), writes=[bd.b])
        k.op("pool", lambda e: e.memset(bd[64:128, 64:128], 1.0), writes=[bd.b])
        wq = cx.sb("dl_wq", [128, 8, 128], BF16, es)
        wk = cx.sb("dl_wk", [128, 8, 128], BF16, es)
        wv = cx.sb("dl_wv", [128, 8, 128], BF16, es)
        Kp = cx.sb("dl_kp", [128, S], BF16, es)
        Qp = [cx.sb("dl_q", [128, S], BF16, es) for i in range(2)]
        k.op("pool", lambda e: e.memset(Qp[0][64:128, :], 0.0), writes=[Qp[0].b])
        k.op("pool", lambda e: e.memset(Qp[1][0:64, :], 0.0), writes=[Qp[1].b])
        V = cx.sb("dl_v", [128, 3, NT, 2, 65], BF16, es)
        k.op("pool", lambda e: e.memset(V[:], 1.0), writes=[V.b])
        sq = [cx.sb("dl_sq", [128, 512], BF16, es) for i in range(2)]
        rr = [cx.sb("dl_r", [128, 512], F32, es) for i in range(2)]
        lanes = []
        for ln in range(2):
            lanes.append(dict(
                accs=cx.sb("dl_acc", [65, S], F32, es),
                pt=[cx.sb("dl_pt", [128, 512], BF16, es) for i in range(2)],
                rl=cx.sb("dl_rl", [128, 512], F32, es),
                ob=[cx.sb("dl_ob", [64, 512], BF16, es) for i in range(2)],
                pq=[cx.ps("dl_pq", [128, 512], F32, es) for i in range(2)],
                po=[cx.ps("dl_po", [128, 512], F32, es) for i in range(2)]))

        def attn_gen(h, ln, Q, R):
            accs, pt, rl, pq, po = R["accs"], R["pt"], R["rl"], R["pq"], R["po"]
            steps = []
            nst = 0
            for pi, (win, d) in enumerate(DIL_PATTERNS):
                nb = S // (128 * d)
                per = min(4, nb)
                for r in range(d):
                    for n0 in range(0, nb, per):
                        n2s = list(range(n0, n0 + per, 2))
                        for n2 in n2s:
                            steps.append((pi, d, nb, r, n0, per, n2, min(2, n0 + per - n2), nst, n2 == n2s[-1]))
                        nst += 1

            def stS(j):
                pi, d, nb, r, n0, per, n2, nu, bk, lastb = steps[j]
                pS = pq[j % 2]
                for u in range(nu):
                    n = n2 + u
                    qs = slice(128 * n * d + r, 128 * n * d + r + 127 * d + 1, d)
                    if n > 0:
                        ks = slice(128 * (n - 1) * d + r, 128 * (n - 1) * d + r + 127 * d + 1, d)
                        k.op("pe", lambda e: e.matmul(pS[:, u * 256:u * 256 + 128], lhsT=Kp[:, ks],
                                                      rhs=Q[:, qs], start=True, stop=False),
                             reads=[Kp.b, Q.b], writes=[pS.b])
                        k.op("pe", lambda e: e.matmul(pS[:, u * 256:u * 256 + 128], lhsT=c["id_b"][:],
                                                      rhs=tbias[:, h, pi, 0, :], start=False, stop=True),
                             reads=[tbias.b, c["id_b"].b], writes=[pS.b])
                    k.op("pe", lambda e: e.matmul(pS[:, u * 256 + 128:u * 256 + 256], lhsT=Kp[:, qs],
                                                  rhs=Q[:, qs], start=True, stop=False),
                         reads=[Kp.b, Q.b], writes=[pS.b])
                    k.op("pe", lambda e: e.matmul(pS[:, u * 256 + 128:u * 256 + 256], lhsT=c["id_b"][:],
                                                  rhs=tbias[:, h, pi, 1, :], start=False, stop=True),
                         reads=[tbias.b, c["id_b"].b], writes=[pS.b])

            def stP(j):
                pi, d, nb, r, n0, per, n2, nu, bk, lastb = steps[j]
                pS, P, pO = pq[j % 2], pt[j % 2], po[bk % 2]
                lo = 128 if n2 == 0 else 0
                k.op("act", lambda e: e.activation(out=P[:, lo:256 * nu], in_=pS[:, lo:256 * nu],
                                                   func=AF.Exp), reads=[pS.b], writes=[P.b])
                for u in range(nu):
                    n = n2 + u
                    oc = (n - n0) * 128
                    if n > 0:
                        k.op("pe", lambda e: e.matmul(pO[0:65, oc:oc + 128],
                                                      lhsT=V[:, pi, r * nb + n - 1, ln, :],
                                                      rhs=P[:, u * 256:u * 256 + 128], start=True,
                                                      stop=False), reads=[V.b, P.b], writes=[pO.b])
                    k.op("pe", lambda e: e.matmul(pO[0:65, oc:oc + 128], lhsT=V[:, pi, r * nb + n, ln, :],
                                                  rhs=P[:, u * 256 + 128:u * 256 + 256], start=(n == 0),
                                                  stop=True), reads=[V.b, P.b], writes=[pO.b])
                if lastb:
                    s0 = 128 * n0 * d + r
                    dst = accs[:, s0:s0 + (128 * per - 1) * d + 1:d]
                    if pi == 0:
                        k.op("act", lambda e: e.activation(out=dst, in_=pO[0:65, 0:128 * per], func=AF.Identity),
                             reads=[pO.b], writes=[accs.b])
                    else:
                        k.op("dve", lambda e: e.tensor_tensor(out=dst, in0=dst, in1=pO[0:65, 0:128 * per],
                                                              op=ALU.add), reads=[pO.b], writes=[accs.b])

            stS(0)
            yield
            for j in range(len(steps)):
                if j + 1 < len(steps):
                    stS(j + 1)
                stP(j)
                yield
            pb = pq[0]
            for g in range(NG):
                c0 = g * 512
                O = R["ob"][g % 2]
                k.op("act", lambda e: e.activation(out=rl[64:65, :], in_=accs[64:65, c0:c0 + 512], func=AF.Ln),
                     reads=[accs.b], writes=[rl.b])
                k.op("act", lambda e: e.activation(out=rl[64:65, :], in_=rl[64:65, :], func=AF.Exp, scale=-1.0),
                     writes=[rl.b])
                k.op("pe", lambda e: e.matmul(pb[0:64, :], lhsT=c["ones_f"][64:65, 0:64], rhs=rl[64:65, :],
                                              start=True, stop=True), reads=[rl.b, c["ones_f"].b], writes=[pb.b])
                k.op("dve", lambda e: e.tensor_tensor(out=O[:], in0=accs[0:64, c0:c0 + 512], in1=pb[0:64, :],
                                                      op=ALU.mult), reads=[accs.b, pb.b], writes=[O.b])
                k.dma("sp", mixedT[MOFF + 64 * h:MOFF + 64 * h + 64, c0:c0 + 512], O[:], reads=[O.b],
                      writes=[mixedT.b])
                yield

        for pr_ in range(3):
            load_w(cx, wq, w_in, L, QOFF + 128 * pr_, 128)
            load_w(cx, wk, w_in, L, KOFF + 128 * pr_, 128)
            load_w(cx, wv, w_in, L, VOFF + 128 * pr_, 128)
            n = 0
            for which in range(2):
                W, gain = (wq, gq) if which == 0 else (wk, gk)
                for tg in range(NG):
                    cs = slice(tg * 512, (tg + 1) * 512)
                    p, pss = lanes[0]["pq"][tg % 2], lanes[1]["pq"][tg % 2]
                    s_, r_ = sq[n % 2], rr[n % 2]
                    n += 1
                    for kk in range(8):
                        k.op("pe", lambda e: e.matmul(p[:], lhsT=W[:, kk, :], rhs=hT[:, kk, cs],
                                                      start=(kk == 0), stop=(kk == 7)),
                             reads=[W.b, hT.b], writes=[p.b])
                    k.op("act", lambda e: e.activation(out=s_[:], in_=p[:], func=AF.Square), reads=[p.b], writes=[s_.b])
                    k.op("pe", lambda e: e.matmul(pss[:], lhsT=bd[:], rhs=s_[:], start=True, stop=True),
                         reads=[s_.b, bd.b], writes=[pss.b])
                    k.op("act", lambda e: e.activation(out=r_[:], in_=pss[:], func=AF.Ln, scale=1.0 / HD,
                                                       bias=c["eps"][:]), reads=[pss.b, c["eps"].b], writes=[r_.b])
                    k.op("act", lambda e: e.activation(out=r_[:], in_=r_[:], func=AF.Exp, scale=-0.5), writes=[r_.b])
                    if which == 0:
                        for half in range(2):
                            hs = slice(64 * half, 64 * half + 64)
                            k.op("dve", lambda e: e.scalar_tensor_tensor(out=Qp[half][hs, cs], in0=p[hs, :],
                                                                         scalar=gain[hs, :], in1=r_[hs, :],
                                                                         op0=ALU.mult, op1=ALU.mult),
                                 reads=[p.b, r_.b, gain.b], writes=[Qp[half].b])
                    else:
                        k.op("dve", lambda e: e.scalar_tensor_tensor(out=Kp[:, cs], in0=p[:], scalar=gain[:],
                                                                     in1=r_[:], op0=ALU.mult, op1=ALU.mult),
                             reads=[p.b, r_.b, gain.b], writes=[Kp.b])
            nv = 0
            for pi, (win, d) in enumerate(DIL_PATTERNS):
                nb = S // (128 * d)
                tiles = [(r, n) for r in range(d) for n in range(nb)]
                for t4 in range(0, len(tiles), 4):
                    p = lanes[nv % 2]["po"][(nv // 2) % 2]
                    nv += 1
                    for jj, (r, n) in enumerate(tiles[t4:t4 + 4]):
                        s0 = 128 * n * d + r
                        for kk in range(8):
                            k.op("pe", lambda e: e.matmul(p[:, jj * 128:(jj + 1) * 128],
                                                          lhsT=hT[:, kk, s0:s0 + 127 * d + 1:d], rhs=wv[:, kk, :],
                                                          start=(kk == 0), stop=(kk == 7)),
                                 reads=[hT.b, wv.b], writes=[p.b])
                    k.op("act", lambda e: e.activation(out=V[:, pi, t4:t4 + 4, :, 0:64],
                                                       in_=p[:].rearrange("p (j h e) -> p j h e", j=4, h=2),
                                                       func=AF.Identity), reads=[p.b], writes=[V.b])
            run_lanes([attn_gen(2 * pr_ + ln, ln, Qp[ln], lanes[ln]) for ln in range(2)])
        k.barrier()


def emit_wout(cx, c, L, modb, xsrc, xdst, w_out, mixedT):
    k = cx.k
    with contextlib.ExitStack() as es:
        wo = cx.sb("wo_w", [128, 8, D], BF16, es)
        mT = [cx.sb("wo_m%d" % i, [128, 8, 512], BF16, es) for i in range(2)]
        xin = [cx.sb("wo_x%d" % i, [128, D], F32, es) for i in range(3)]
        xo = [cx.sb("wo_o%d" % i, [128, D], F32, es) for i in range(3)]
        pw = [cx.ps("wo_p%d" % i, [128, 512], F32, es) for i in range(4)]
        k.dma("pool", wo[:], w_out[L].rearrange("(k p) d -> p k d", p=128), writes=[wo.b])
        for g in range(cx.S // 512):
            m = mT[g % 2]
            k.dma("sp", m[:], mixedT[:, g * 512:(g + 1) * 512].rearrange("(k p) t -> p k t", p=128),
                  reads=[mixedT.b], writes=[m.b])
            for tt in range(4):
                i = g * 4 + tt
                x, o = xin[i % 3], xo[i % 3]
                k.dma("sp", x[:], xsrc(i), reads=[xsrc.b], writes=[x.b])
                for dh in range(2):
                    p = pw[(i % 2) * 2 + dh]
                    for kk in range(8):
                        k.op("pe", lambda e: e.matmul(p[:], lhsT=m[:, kk, tt * 128:(tt + 1) * 128],
                                                      rhs=wo[:, kk, dh * 512:(dh + 1) * 512],
                                                      start=(kk == 0), stop=(kk == 7)),
                             reads=[m.b, wo.b], writes=[p.b])
                    k.op("dve", lambda e: e.tensor_tensor(out=o[:, dh * 512:(dh + 1) * 512], in0=p[:],
                                                          in1=modb[:, 2048 + dh * 512:2048 + (dh + 1) * 512],
                                                          op=ALU.mult), reads=[p.b, modb.b], writes=[o.b])
                k.op("pool", lambda e: e.tensor_tensor(out=o[:], in0=o[:], in1=x[:], op=ALU.add),
                     reads=[x.b], writes=[o.b])
                k.dma("act", xdst(i), o[:], reads=[o.b], writes=[xdst.b])
        k.barrier()


def emit_mixer(cx, c, L, modb, xsrc, xdst, P, mixedT, parts=("fox", "dil", "sb")):
    if len(parts) < 3:
        xdst = None
    k = cx.k
    with contextlib.ExitStack() as es:
        hT = cx.sb("hT", [128, 8, cx.S], BF16, es)
        with contextlib.ExitStack() as es2:
            emit_norm(cx, c, es2, xsrc, modb, 1024, 0, hT, range(cx.NT), 0)
            k.barrier()
        if "fox" in parts:
            emit_fox(cx, c, L, hT, P["w_in"], P["b_forget"], P["q_gain_fox"], P["k_gain_fox"], mixedT)
        if "dil" in parts:
            emit_dil(cx, c, L, hT, P["w_in"], P["q_gain_dil"], P["k_gain_dil"], P["tb_gather"], P["tb_mask"], mixedT)
        if "sb" in parts:
            emit_sb(cx, c, L, hT, P["w_in"], mixedT)
        k.barrier()
    if xdst is not None:
        emit_wout(cx, c, L, modb, xsrc, xdst, P["w_out"], mixedT)


def emit_ffn_sparse(cx, c, L, modb, xsrc, xdst, router_w, router_b, w_gate, w_up, w_down, scr):
    k = cx.k
    NT = cx.NT
    NBT = cx.S // 512
    NB = NBT + 3
    I32 = mybir.dt.int32
    h2s, combs, ys = scr["h2s"], scr["combs"], scr["ys"]
    wg_rows = w_gate.rearrange("l e (p h k) f -> (l e p h) (k f)", h=4, k=2)
    wu_rows = w_up.rearrange("l e (p h k) f -> (l e p h) (k f)", h=4, k=2)
    wd_rows = w_down.rearrange("l e (p h c) d -> (l e p h) (c d)", h=4, c=1)
    with contextlib.ExitStack() as es:
        pos_i = cx.sb("s_posi", [128, NT], I32, es)
        idxE = cx.sb("s_idxE", [128, NB, 16], I32, es)
        zer = cx.sb("s_zero", [128, 256], F32, es)
        k.op("pool", lambda e: e.memset(zer[:], 0.0), writes=[zer.b])
        k.dma("pool", combs[:, :].rearrange("(p r) c -> p (r c)", p=128), zer[:, 0:NB * 16], reads=[zer.b],
              writes=[combs.b])
        with contextlib.ExitStack() as es1:
            h2tok = cx.sb("s_h2tok", [128, NT, D], BF16, es1)
            rw = cx.sb("f_rw", [128, 8, 16], F32, es1)
            rb = cx.sb("f_rb", [128, 16], F32, es1)
            rb1 = cx.sb("f_rb1", [1, 16], F32, es1)
            logits = cx.sb("f_logits", [128, NT, 16], F32, es1)
            comb = cx.sb("f_comb", [128, NT, 16], F32, es1)
            G = cx.sb("s_G", [128, NT, 4], F32, es1)
            combg = cx.sb("s_combg", [128, NT, 4], F32, es1)
            k.dma("sp", rw[:], router_w.rearrange("(k p) e -> p k e", p=128), writes=[rw.b])
            k.dma("sp", rb1[:], router_b, writes=[rb1.b])
            with contextlib.ExitStack() as es2:
                prb = cx.ps("f_prb", [128, 16], F32, es2)
                k.op("pe", lambda e: e.matmul(prb[:], lhsT=c["ones_f"][0:1, :], rhs=rb1[0:1, :], start=True, stop=True),
                     reads=[c["ones_f"].b, rb1.b], writes=[prb.b])
                k.op("act", lambda e: e.activation(out=rb[:], in_=prb[:], func=AF.Identity), reads=[prb.b],
                     writes=[rb.b])
                k.barrier()
            with contextlib.ExitStack() as es2:
                emit_norm(cx, c, es2, xsrc, modb, 4096, 3072, None, range(NT), 0,
                          router=dict(rw=rw, rb=rb, logits=logits), rows_dst=h2tok)
                k.barrier()
            with contextlib.ExitStack() as es2:
                emit_router(cx, es2, logits, comb, 0, NT, G=G, combg=combg)
                Gb = cx.sb("s_Gb", [128, NT * 4], BF16, es2)
                tri = cx.sb("s_tri", [128, 128], BF16, es2)
                cnt = cx.sb("s_cnt", [128, NT, 4], F32, es2)
                inc = cx.sb("s_inc", [128, NT, 4], F32, es2)
                tot = cx.sb("s_tot", [128, NT, 4], F32, es2)
                thr = cx.sb("s_thr", [128, NB], F32, es2)
                thr_i = cx.sb("s_thri", [128, NB], I32, es2)
                cmp_ = cx.sb("s_cmp", [128, 4, NBT], F32, es2)
                nbg = cx.sb("s_nbg", [128, 4], F32, es2)
                po = cx.sb("s_po", [128, 4], F32, es2)
                cmpb = cx.sb("s_cmpb", [128, NB, 3], F32, es2)
                gb = cx.sb("s_gb", [128, NB], F32, es2)
                posf = cx.sb("s_posf", [128, NT], F32, es2)
                cE_i = cx.sb("s_cEi", [128, 16], I32, es2)
                cE = cx.sb("s_cE", [128, 16], F32, es2)
                fE = cx.sb("s_fE", [128, NB, 16], F32, es2)
                prk = cx.ps("s_prk", [128, NT * 4], F32, es2)
                pcn = cx.ps("s_pcn", [128, NT * 4], F32, es2)
                k.op("pool", lambda e: e.affine_select(out=tri[:], in_=c["ones_b"][:], pattern=[[1, 128]],
                                                       compare_op=ALU.is_gt, fill=0.0, base=0, channel_multiplier=-1),
                     reads=[c["ones_b"].b], writes=[tri.b])
                k.op("pool", lambda e: e.iota(thr_i[:], pattern=[[512, NB]], base=0, channel_multiplier=0),
                     writes=[thr_i.b])
                k.op("dve", lambda e: e.tensor_copy(out=thr[:], in_=thr_i[:]), reads=[thr_i.b], writes=[thr.b])
                k.op("pool", lambda e: e.iota(cE_i[:], pattern=[[512, 4], [1, 4]], base=L * NEXP * 512, channel_multiplier=4),
                     writes=[cE_i.b])
                k.op("dve", lambda e: e.tensor_copy(out=cE[:], in_=cE_i[:]), reads=[cE_i.b], writes=[cE.b])
                k.op("dve", lambda e: e.tensor_copy(out=Gb[:], in_=G[:].rearrange("p n g -> p (n g)")),
                     reads=[G.b], writes=[Gb.b])
                k.op("pe", lambda e: e.matmul(prk[:], lhsT=tri[:], rhs=Gb[:], start=True, stop=True),
                     reads=[tri.b, Gb.b], writes=[prk.b])
                k.op("pe", lambda e: e.matmul(pcn[:], lhsT=c["ones_b"][:], rhs=Gb[:], start=True, stop=True),
                     reads=[c["ones_b"].b, Gb.b], writes=[pcn.b])
                k.op("act", lambda e: e.activation(out=cnt[:].rearrange("p n g -> p (n g)"), in_=pcn[:],
                                                   func=AF.Identity), reads=[pcn.b], writes=[cnt.b])
                for g in range(4):
                    k.op("dve", lambda e: e.tensor_tensor_scan(out=inc[:, :, g], data0=cnt[:, :, g], data1=cnt[:, :, g],
                                                               initial=0.0, op0=ALU.add, op1=ALU.max),
                         reads=[cnt.b], writes=[inc.b])
                k.op("dve", lambda e: e.tensor_tensor(out=cmp_[:],
                                                      in0=inc[:, NT - 1, :].unsqueeze(2).to_broadcast([128, 4, NBT]),
                                                      in1=thr[:, 0:NBT].unsqueeze(1).to_broadcast([128, 4, NBT]),
                                                      op=ALU.is_gt), reads=[inc.b, thr.b], writes=[cmp_.b])
                k.op("dve", lambda e: e.tensor_reduce(out=nbg[:], in_=cmp_[:], axis=AX.X, op=ALU.add),
                     reads=[cmp_.b], writes=[nbg.b])
                k.op("pool", lambda e: e.memset(po[:], 0.0), writes=[po.b])
                for g in range(1, 4):
                    k.op("dve", lambda e: e.scalar_tensor_tensor(out=po[:, g:g + 1], in0=nbg[:, g - 1:g], scalar=512.0,
                                                                 in1=po[:, g - 1:g], op0=ALU.mult, op1=ALU.add),
                         reads=[nbg.b], writes=[po.b])
                k.op("dve", lambda e: e.tensor_tensor(out=tot[:], in0=inc[:], in1=cnt[:], op=ALU.subtract),
                     reads=[inc.b, cnt.b], writes=[tot.b])
                k.op("dve", lambda e: e.tensor_tensor(out=tot[:], in0=tot[:],
                                                      in1=po[:].unsqueeze(1).to_broadcast([128, NT, 4]), op=ALU.add),
                     reads=[po.b], writes=[tot.b])
                k.op("dve", lambda e: e.tensor_tensor(out=tot[:].rearrange("p n g -> p (n g)"),
                                                      in0=tot[:].rearrange("p n g -> p (n g)"), in1=prk[:], op=ALU.add),
                     reads=[prk.b], writes=[tot.b])
                k.op("dve", lambda e: e.tensor_tensor(out=tot[:], in0=tot[:], in1=G[:], op=ALU.mult),
                     reads=[G.b], writes=[tot.b])
                k.op("dve", lambda e: e.tensor_reduce(out=posf[:], in_=tot[:], axis=AX.X, op=ALU.add),
                     reads=[tot.b], writes=[posf.b])
                k.op("dve", lambda e: e.tensor_copy(out=pos_i[:], in_=posf[:]), reads=[posf.b], writes=[pos_i.b])
                k.op("dve", lambda e: e.tensor_tensor(out=cmpb[:],
                                                      in0=po[:, 1:4].unsqueeze(1).to_broadcast([128, NB, 3]),
                                                      in1=thr[:].unsqueeze(2).to_broadcast([128, NB, 3]),
                                                      op=ALU.is_le), reads=[po.b, thr.b], writes=[cmpb.b])
                k.op("dve", lambda e: e.tensor_reduce(out=gb[:], in_=cmpb[:], axis=AX.X, op=ALU.add),
                     reads=[cmpb.b], writes=[gb.b])
                k.op("dve", lambda e: e.scalar_tensor_tensor(out=fE[:], in0=gb[:].unsqueeze(2).to_broadcast([128, NB, 16]),
                                                             scalar=2048.0,
                                                             in1=cE[:].unsqueeze(1).to_broadcast([128, NB, 16]),
                                                             op0=ALU.mult, op1=ALU.add),
                     reads=[gb.b, cE.b], writes=[fE.b])
                k.op("dve", lambda e: e.tensor_copy(out=idxE[:], in_=fE[:]), reads=[fE.b], writes=[idxE.b])
                for i in range(NT):
                    k.dma("pool", None, None, reads=[h2tok.b, pos_i.b], writes=[h2s.b], nowaw=True,
                          fn=lambda eng: eng.indirect_dma_start(
                              out=h2s[:, :], out_offset=bass.IndirectOffsetOnAxis(ap=pos_i[:, i:i + 1], axis=0),
                              in_=h2tok[:, i, :], in_offset=None))
                    k.dma("pool", None, None, reads=[combg.b, pos_i.b], writes=[combs.b], nowaw=True,
                          fn=lambda eng: eng.indirect_dma_start(
                              out=combs[:, :], out_offset=bass.IndirectOffsetOnAxis(ap=pos_i[:, i:i + 1], axis=0),
                              in_=combg[:, i, :], in_offset=None))
                k.barrier()
        with contextlib.ExitStack() as es1:
            rows = [cx.sb("s_rows", [128, 4, D], BF16, es1) for i in range(2)]
            hTb = [cx.sb("s_hT", [128, 8, 512], BF16, es1) for i in range(2)]
            cmb = [cx.sb("s_cmb", [128, 4, 4], F32, es1) for i in range(2)]
            yb = [cx.sb("s_y", [128, 4, D], F32, es1) for i in range(2)]
            NW = 3
            wg = [cx.sb("m_wg", [128, 8, DFF], BF16, es1) for i in range(NW)]
            wu = [cx.sb("m_wu", [128, 8, DFF], BF16, es1) for i in range(NW)]
            wd = [cx.sb("m_wd", [128, 4, D], BF16, es1) for i in range(NW)]
            sg = [cx.sb("m_sg", [128, 512], F32, es1) for i in range(2)]
            hid = [cx.sb("m_hid", [128, 4, 512], BF16, es1) for i in range(2)]
            pg = [cx.ps("m_pg", [128, 512], F32, es1) for i in range(2)]
            pu = [cx.ps("m_pu", [128, 512], F32, es1) for i in range(2)]
            pd = [cx.ps("m_pd", [128, 512], F32, es1) for i in range(2)]
            ptr = [cx.ps("s_ptr", [128, 8, 128], BF16, es1) for i in range(2)]

            def load_expert(b, j, n):
                i = n % NW
                for hh in range(4):
                    off = bass.IndirectOffsetOnAxis(ap=idxE[:, b, 4 * j + hh:4 * j + hh + 1], axis=0)
                    k.dma("pool", None, None, reads=[idxE.b], writes=[wg[i].b], nowaw=True,
                          fn=lambda eng: eng.indirect_dma_start(
                              out=wg[i][:, 2 * hh:2 * hh + 2, :].rearrange("p k f -> p (k f)"), out_offset=None,
                              in_=wg_rows, in_offset=off))
                    k.dma("pool", None, None, reads=[idxE.b], writes=[wu[i].b], nowaw=True,
                          fn=lambda eng: eng.indirect_dma_start(
                              out=wu[i][:, 2 * hh:2 * hh + 2, :].rearrange("p k f -> p (k f)"), out_offset=None,
                              in_=wu_rows, in_offset=off))
                    k.dma("pool", None, None, reads=[idxE.b], writes=[wd[i].b], nowaw=True,
                          fn=lambda eng: eng.indirect_dma_start(
                              out=wd[i][:, hh, :], out_offset=None,
                              in_=wd_rows, in_offset=off))

            def prep_block(b):
                R_, H_, C_ = rows[b % 2], hTb[b % 2], cmb[b % 2]
                k.dma("sp", R_[:], h2s[b * 512:(b + 1) * 512, :].rearrange("(j p) d -> p j d", p=128),
                      reads=[h2s.b], writes=[R_.b])
                k.dma("sp", C_[:], combs[b * 512:(b + 1) * 512, :].rearrange("(j p) c -> p j c", p=128),
                      reads=[combs.b], writes=[C_.b])
                for tt in range(4):
                    p = ptr[tt % 2]
                    for kk in range(8):
                        k.op("pe", lambda e: e.transpose(p[:, kk, :], R_[:, tt, kk:kk + 1017:8],
                                                         c["id_b"][:]), reads=[R_.b, c["id_b"].b], writes=[p.b])
                    k.op("dve", lambda e: e.tensor_copy(out=H_[:, :, tt * 128:(tt + 1) * 128], in_=p[:]),
                         reads=[p.b], writes=[H_.b])

            work = [(b, j) for b in range(NB) for j in range(4)]
            load_expert(0, 0, 0)
            load_expert(work[1][0], work[1][1], 1)
            for n, (b, j) in enumerate(work):
                if n + 2 < len(work):
                    load_expert(work[n + 2][0], work[n + 2][1], n + 2)
                R_, H_, C_, Y_ = rows[b % 2], hTb[b % 2], cmb[b % 2], yb[b % 2]
                if n == 0:
                    prep_block(0)
                if j == 2 and b + 1 < NB:
                    prep_block(b + 1)
                g_, u_, d_ = wg[n % NW], wu[n % NW], wd[n % NW]
                hd = hid[n % 2]
                for fc in range(4):
                    pG, pU, s_ = pg[fc % 2], pu[fc % 2], sg[fc % 2]
                    for kk in range(8):
                        k.op("pe", lambda en: en.matmul(pG[:], lhsT=g_[:, kk, fc:fc + 509:4],
                                                        rhs=H_[:, kk, :], start=(kk == 0), stop=(kk == 7)),
                             reads=[g_.b, H_.b], writes=[pG.b])
                    for kk in range(8):
                        k.op("pe", lambda en: en.matmul(pU[:], lhsT=u_[:, kk, fc:fc + 509:4],
                                                        rhs=H_[:, kk, :], start=(kk == 0), stop=(kk == 7)),
                             reads=[u_.b, H_.b], writes=[pU.b])
                    k.op("act", lambda en: en.activation(out=s_[:], in_=pG[:], func=AF.Silu),
                         reads=[pG.b], writes=[s_.b])
                    k.op("dve", lambda en: en.tensor_tensor(out=hd[:, fc, :], in0=pU[:], in1=s_[:], op=ALU.mult),
                         reads=[pU.b, s_.b], writes=[hd.b])
                for tt in range(4):
                    for dh in range(2):
                        pD = pd[dh]
                        for fc in range(4):
                            k.op("pe", lambda en: en.matmul(pD[:], lhsT=hd[:, fc, tt * 128:(tt + 1) * 128],
                                                            rhs=d_[:, fc, dh * 512:(dh + 1) * 512],
                                                            start=(fc == 0), stop=(fc == 3)),
                                 reads=[hd.b, d_.b], writes=[pD.b])
                        a = Y_[:, tt, dh * 512:(dh + 1) * 512]
                        if j == 0:
                            k.op("dve", lambda en: en.tensor_scalar(out=a, in0=pD[:], scalar1=C_[:, tt, j:j + 1],
                                                                    scalar2=None, op0=ALU.mult),
                                 reads=[pD.b, C_.b], writes=[Y_.b])
                        else:
                            k.op("dve", lambda en: en.scalar_tensor_tensor(out=a, in0=pD[:], scalar=C_[:, tt, j:j + 1],
                                                                           in1=a, op0=ALU.mult, op1=ALU.add),
                                 reads=[pD.b, C_.b], writes=[Y_.b])
                if j == 3:
                    k.dma("sp", ys[b * 512:(b + 1) * 512, :].rearrange("(j p) d -> p j d", p=128), Y_[:],
                          reads=[Y_.b], writes=[ys.b])
            k.barrier()
        with contextlib.ExitStack() as es1:
            xin = [cx.sb("o_xin", [128, D], F32, es1) for i in range(4)]
            yg = [cx.sb("o_yg", [128, D], F32, es1) for i in range(4)]
            def fetch(i):
                x, y_ = xin[i % 4], yg[i % 4]
                k.dma("sp", x[:], xsrc(i), reads=[xsrc.b], writes=[x.b])
                k.dma("pool", None, None, reads=[ys.b, pos_i.b], writes=[y_.b],
                      fn=lambda eng: eng.indirect_dma_start(
                          out=y_[:], out_offset=None, in_=ys[:, :],
                          in_offset=bass.IndirectOffsetOnAxis(ap=pos_i[:, i:i + 1], axis=0)))

            for i in range(min(3, NT)):
                fetch(i)
            for i in range(NT):
                x, y_ = xin[i % 4], yg[i % 4]
                k.op("dve", lambda e: e.tensor_tensor(out=y_[:], in0=y_[:], in1=modb[:, 5120:6144], op=ALU.mult),
                     reads=[modb.b], writes=[y_.b])
                k.op("dve", lambda e: e.tensor_tensor(out=y_[:], in0=y_[:], in1=x[:], op=ALU.add),
                     reads=[x.b], writes=[y_.b])
                k.dma("act", xdst(i), y_[:], reads=[y_.b], writes=[xdst.b])
                if i + 3 < NT:
                    fetch(i + 3)
            k.barrier()


class TileSrc:
    def __init__(self, t):
        self.t = t
        self.b = t.b

    def __call__(self, i):
        return self.t[i * 128:(i + 1) * 128, :]


def build(S=4096, layers=(0, 1), dbg=None, phases=("mix", "ffn")):
    nc = bass.Bass("TRN2", target_bir_lowering=False)
    es = contextlib.ExitStack()
    with es:
        cx = Ctx(nc, es, S, dbg or {})
        k = cx.k

        def inp(name, shape):
            return nc.dram_tensor(name, list(shape), F32, kind="ExternalInput").ap()

        x = T(inp("x", [S, D]), k.buf("x"))
        cT = inp("cT", [128, 8])
        ada_w = inp("ada_w", [DEPTH, D, 6 * D])
        ada_b = inp("ada_b", [DEPTH, 6 * D])
        norm_mix = inp("norm_mix", [DEPTH, D])
        norm_ffn = inp("norm_ffn", [DEPTH, D])
        router_w = inp("router_w", [D, NEXP])
        router_b = inp("router_b", [1, NEXP])
        w_gate = inp("w_gate", [DEPTH, NEXP, D, DFF])
        w_up = inp("w_up", [DEPTH, NEXP, D, DFF])
        w_down = inp("w_down", [DEPTH, NEXP, DFF, D])
        P = dict(w_in=inp("w_in", [DEPTH, D, IN_COLS]), b_forget=inp("b_forget", [DEPTH, 4]),
                 q_gain_fox=inp("q_gain_fox", [DEPTH, HD]), k_gain_fox=inp("k_gain_fox", [DEPTH, HD]),
                 q_gain_dil=inp("q_gain_dil", [DEPTH, HD]), k_gain_dil=inp("k_gain_dil", [DEPTH, HD]),
                 w_out=inp("w_out", [DEPTH, D, D]))
        tb_gather = inp("tb_gather", [128, 6, 768])
        tb_mask = inp("tb_mask", [128, 768])
        out = T(nc.dram_tensor("out", [S, D], F32, kind="ExternalOutput").ap(), k.buf("out"))
        xres = cx.dram("xres", [S, D], F32)
        xmid = cx.dram("xmid", [S, D], F32)
        if "mixedT" in cx.dbg:
            mixedT = T(nc.dram_tensor("mixedT", [D, S], BF16, kind="ExternalOutput").ap(), k.buf("mixedT"))
        else:
            mixedT = cx.dram("mixedT", [D, S], BF16)

        c = emit_consts(cx)
        modb = cx.sb("modb", [128, 6 * D], F32)
        sparse = "dense" not in cx.dbg
        if sparse:
            NB = S // 512 + 3
            scr = dict(h2s=cx.dram("h2s", [NB * 512, D], BF16), combs=cx.dram("combs", [NB * 512, 4], F32),
                       ys=cx.dram("ys", [NB * 512, D], F32))
            with contextlib.ExitStack() as es2:
                zb = cx.sb("zb", [128, 4 * D], BF16, es2)
                k.op("pool", lambda e: e.memset(zb[:], 0.0), writes=[zb.b])
                for b in range(NB):
                    k.dma("pool", scr["h2s"][b * 512:(b + 1) * 512, :].rearrange("(p r) d -> p (r d)", p=128), zb[:],
                          reads=[zb.b], writes=[scr["h2s"].b])
                k.barrier()
        P["tb_gather"], P["tb_mask"] = tb_gather, tb_mask
        cur = x
        for li, L in enumerate(layers):
            last = li == len(layers) - 1
            emit_mod(cx, c, L, modb, cT, ada_w, ada_b, norm_mix, norm_ffn)
            dst = out if last else xres
            if "mix" in phases:
                mdst = xmid if "ffn" in phases else dst
                emit_mixer(cx, c, L, modb, TileSrc(cur), TileSrc(mdst), P, mixedT, parts=cx.dbg.get("parts", ("fox", "dil", "sb")))
                cur = mdst
            if "ffn" in phases and sparse:
                emit_ffn_sparse(cx, c, L, modb, TileSrc(cur), TileSrc(dst), router_w, router_b, w_gate, w_up, w_down,
                                scr)
            elif "ffn" in phases:
                emit_ffn(cx, c, L, modb, TileSrc(cur), TileSrc(dst), router_w, router_b, w_gate, w_up, w_down)
            cur = dst
        k.finish([out.b])
    return nc


def dil_bias_tables(rel_bias):
    kk = np.arange(128)[:, None]
    qq = np.arange(128)[None, :]
    idx = np.zeros((3, 2, 128, 128), np.int64)
    msk = np.zeros((3, 2, 128, 128), np.float32)
    for pi, (win, d) in enumerate(DIL_PATTERNS):
        span = win // d
        for cc in range(2):
            steps = qq + 128 - (kk + 128 * cc)
            band = (steps >= 0) & (steps <= span)
            dist = np.maximum(steps, 0) * d
            dd = np.maximum(dist, 16).astype(np.float64)
            large = 16 + (np.log(dd / 16.0) / np.log(2048.0 / 16.0) * 16.0).astype(np.int64)
            large = np.minimum(large, 31)
            bucket = np.where(dist < 16, dist, large)
            idx[pi, cc] = np.where(band, bucket, 0)
            msk[pi, cc] = np.where(band, 0.0, NEG)
    g = rel_bias[idx]
    g = np.ascontiguousarray(np.transpose(g, (2, 4, 0, 1, 3))).reshape(128, 6, 768)
    m = np.ascontiguousarray(np.transpose(msk, (2, 0, 1, 3))).reshape(128, 768)
    return g.astype(np.float32), m


def make_in_maps(inputs, S=4096):
    f = lambda a: np.ascontiguousarray(np.asarray(a, dtype=np.float32))
    shared = {n: f(inputs[n]) for n in ("ada_w", "ada_b", "norm_mix", "norm_ffn", "router_w",
                                        "w_gate", "w_up", "w_down", "w_in", "b_forget", "q_gain_fox",
                                        "k_gain_fox", "q_gain_dil", "k_gain_dil", "w_out")}
    shared["tb_gather"], shared["tb_mask"] = dil_bias_tables(f(inputs["rel_bias"]))
    shared["router_b"] = f(inputs["router_b"]).reshape(1, NEXP)
    maps = []
    for b in range(NCORES):
        m = dict(shared)
        m["x"] = f(inputs["x"][b])
        m["cT"] = np.ascontiguousarray(f(inputs["c"][b]).reshape(8, 128).T)
        maps.append(m)
    return maps


def kernel(**inputs):
    nc = build()
    in_maps = make_in_maps(inputs)
    res = run_bass_kernel_spmd(nc, in_maps, core_ids=list(range(NCORES)))
    return np.stack([np.asarray(r["out"]) for r in res.results], axis=0).astype(np.float32)
```
